# Optimizing a Trainium2 kernel written in Bass

```python
import jax, jax.numpy as jnp
from jax import lax
import numpy as np

D_MODEL = 2048
BATCH = 4
SEQ = 8192
DEPTH = 2
DEC_BATCH = 16
DEC_SEQ = 16
PAST_LEN = 1024

CHUNK = 64
N_HEADS = 32
HEAD_DIM = D_MODEL // N_HEADS
N_LEFT_CHUNKS = 8
ATT_REACH = N_LEFT_CHUNKS * CHUNK
BAND = ATT_REACH + CHUNK
REL_CLIP = 256
D_RNN = D_MODEL
N_RNN_BLOCKS = 8
RNN_BLOCK = D_RNN // N_RNN_BLOCKS
CONV_W = 4
RG_C = 8.0
D_FF = 5632
N_EXPERTS = 8
TOP_K = 2
MOE_FF = 5632
MOE_BLOCK = 512
N_ATT_LAYERS = (DEPTH + 1) // 2
N_RNN_LAYERS = DEPTH // 2
N_DENSE_FFN = (DEPTH + 1) // 2
N_MOE_FFN = DEPTH // 2
ALPHA = (2.0 * DEPTH) ** 0.25
BETA = (8.0 * DEPTH) ** -0.25
LN_EPS = 1e-5
NEG_INF = -1e30

kernel_name = 'hybrid_chunk_attn_rglru_stream_step'


def layer_norm(x, g, b):
    xf = x.astype(jnp.float32)
    mu = jnp.mean(xf, axis=-1, keepdims=True)
    var = jnp.mean(jnp.square(xf - mu), axis=-1, keepdims=True)
    return ((xf - mu) * lax.rsqrt(var + LN_EPS) * g.astype(jnp.float32) + b.astype(jnp.float32)).astype(x.dtype)


def qkv_heads(x, w_in):
    b, t, _ = x.shape
    qkv = jnp.einsum('btd,de->bte', x, w_in).reshape(b, t, 3, N_HEADS, HEAD_DIM)
    return qkv[:, :, 0], qkv[:, :, 1], qkv[:, :, 2]


def band_attention(q, k, v, q_pos, k_pos, rel_bias):
    s = jnp.einsum('bqhd,bkhd->bhqk', q.astype(jnp.float32), k.astype(jnp.float32)) * (HEAD_DIM ** -0.5)
    rel = jnp.clip(q_pos[:, None] - k_pos[None, :], -REL_CLIP, REL_CLIP) + REL_CLIP
    s = s + rel_bias.astype(jnp.float32)[:, rel][None]
    dc = q_pos[:, None] // CHUNK - k_pos[None, :] // CHUNK
    mask = (k_pos[None, :] >= 0) & (dc >= 0) & (dc <= N_LEFT_CHUNKS)
    s = jnp.where(mask[None, None], s, NEG_INF)
    p = jax.nn.softmax(s, axis=-1)
    return jnp.einsum('bhqk,bkhd->bqhd', p.astype(v.dtype), v)


def chunk_attention_prompt(q, k, v, rel_bias):
    b, s = q.shape[:2]
    pad = ((0, 0), (ATT_REACH, 0), (0, 0), (0, 0))
    k_pad = jnp.pad(k, pad)
    v_pad = jnp.pad(v, pad)
    offs_q = jnp.arange(CHUNK, dtype=jnp.int32)
    offs_k = jnp.arange(BAND, dtype=jnp.int32) - ATT_REACH

    def one_chunk(c):
        start = c * CHUNK
        qc = lax.dynamic_slice_in_dim(q, start, CHUNK, axis=1)
        kc = lax.dynamic_slice_in_dim(k_pad, start, BAND, axis=1)
        vc = lax.dynamic_slice_in_dim(v_pad, start, BAND, axis=1)
        return band_attention(qc, kc, vc, start + offs_q, start + offs_k, rel_bias)

    out = lax.map(one_chunk, jnp.arange(s // CHUNK, dtype=jnp.int32))
    return jnp.moveaxis(out, 0, 1).reshape(b, s, N_HEADS, HEAD_DIM)


def chunk_attention_sample(q, k_new, v_new, cache_k, cache_v, rel_bias):
    n_cache = cache_k.shape[1]
    t = q.shape[1]
    k = jnp.concatenate([cache_k.astype(k_new.dtype), k_new], axis=1)
    v = jnp.concatenate([cache_v.astype(v_new.dtype), v_new], axis=1)
    q_pos = PAST_LEN + jnp.arange(t, dtype=jnp.int32)
    k_pos = PAST_LEN - n_cache + jnp.arange(n_cache + t, dtype=jnp.int32)
    return band_attention(q, k, v, q_pos, k_pos, rel_bias)


def _lin_combine(left, right):
    a_l, b_l = left
    a_r, b_r = right
    return a_l * a_r, a_r * b_l + b_r


def rglru_mixer(x, conv_state, h0, w_in, conv_w, conv_b, w_a, b_a, w_x, b_x, lam, w_out):
    b, t, _ = x.shape
    u, gate = jnp.split(jnp.einsum('btd,de->bte', x, w_in), 2, axis=-1)
    u_ext = jnp.concatenate([conv_state.astype(u.dtype), u], axis=1)
    uc = conv_b
    for j in range(CONV_W):
        uc = uc + u_ext[:, j:j + t] * conv_w[j]
    ub = uc.reshape(b, t, N_RNN_BLOCKS, RNN_BLOCK)
    r = jax.nn.sigmoid((jnp.einsum('btnc,nce->btne', ub, w_a) + b_a).astype(jnp.float32)).reshape(b, t, D_RNN)
    i = jax.nn.sigmoid((jnp.einsum('btnc,nce->btne', ub, w_x) + b_x).astype(jnp.float32)).reshape(b, t, D_RNN)
    log_a = -RG_C * r * jax.nn.softplus(-lam.astype(jnp.float32))
    a = jnp.exp(log_a)
    bterm = jnp.sqrt(-jnp.expm1(2.0 * log_a)) * (i * uc.astype(jnp.float32))
    bterm = bterm.at[:, 0].add(a[:, 0] * h0.astype(jnp.float32))
    _, h = lax.associative_scan(_lin_combine, (a, bterm), axis=1)
    y = jnp.einsum('bte,ed->btd', (jax.nn.gelu(gate.astype(jnp.float32)) * h).astype(x.dtype), w_out)
    return y, u_ext[:, -(CONV_W - 1):], h[:, -1].astype(x.dtype)


def swiglu(x, w_in, w_out):
    g, u = jnp.split(x @ w_in, 2, axis=-1)
    return (jax.nn.silu(g) * u) @ w_out


def moe_swiglu(x, w_router, w_in, w_out):
    n_tok = x.shape[0]
    logits = jnp.dot(x.astype(jnp.float32), w_router.astype(jnp.float32))
    top_logit, top_idx = lax.top_k(logits, TOP_K)
    gates = jax.nn.softmax(top_logit, axis=-1).astype(x.dtype)
    n_assign = n_tok * TOP_K
    flat_e = top_idx.reshape(-1)
    flat_tok = jnp.arange(n_assign, dtype=jnp.int32) // TOP_K
    flat_gate = gates.reshape(-1)
    order = jnp.argsort(flat_e)
    e_sorted = flat_e[order]
    counts = jnp.bincount(flat_e, length=N_EXPERTS)
    padded = (counts + MOE_BLOCK - 1) // MOE_BLOCK * MOE_BLOCK
    start = jnp.cumsum(counts) - counts
    ends_pad = jnp.cumsum(padded)
    start_pad = ends_pad - padded
    dest = start_pad[e_sorted] + jnp.arange(n_assign, dtype=jnp.int32) - start[e_sorted]
    n_blocks = -(-n_assign // MOE_BLOCK) + N_EXPERTS
    n_rows = n_blocks * MOE_BLOCK
    row_tok = jnp.full((n_rows,), n_tok, jnp.int32).at[dest].set(flat_tok[order])
    row_gate = jnp.zeros((n_rows,), x.dtype).at[dest].set(flat_gate[order])
    block_start = jnp.arange(n_blocks, dtype=jnp.int32) * MOE_BLOCK
    block_e = jnp.minimum(jnp.searchsorted(ends_pad, block_start, side='right'), N_EXPERTS - 1)
    x_pad = jnp.concatenate([x, jnp.zeros((1, x.shape[1]), x.dtype)], axis=0)
    xb = x_pad[row_tok].reshape(n_blocks, MOE_BLOCK, x.shape[1])

    def expert_block(args):
        xblk, e = args
        return swiglu(xblk, w_in[e], w_out[e])

    yb = lax.map(expert_block, (xb, block_e)).reshape(n_rows, x.shape[1])
    y = jnp.zeros((n_tok + 1, x.shape[1]), x.dtype).at[row_tok].add(yb * row_gate[:, None])
    return y[:n_tok]


def setup_inputs(seed: int = 0) -> dict:
    key = jax.random.key(seed)
    ks = iter(jax.random.split(key, 32))

    def nrm(shape, scale=1.0):
        return jax.random.normal(next(ks), shape, jnp.float32) * scale

    att_cache = min(ATT_REACH, PAST_LEN)
    na, nr = N_ATT_LAYERS, N_RNN_LAYERS
    a0 = jax.random.uniform(next(ks), (nr, D_RNN), jnp.float32, 0.9, 0.999)
    sig = a0 ** (1.0 / RG_C)
    rg_lambda = jnp.log(sig) - jnp.log1p(-sig)
    return {
        'x_prompt': nrm((BATCH, SEQ, D_MODEL)),
        'x_sample': nrm((DEC_BATCH, DEC_SEQ, D_MODEL)),
        'cache_k': nrm((na, DEC_BATCH, att_cache, N_HEADS, HEAD_DIM)),
        'cache_v': nrm((na, DEC_BATCH, att_cache, N_HEADS, HEAD_DIM)),
        'state_conv': nrm((nr, DEC_BATCH, CONV_W - 1, D_RNN)),
        'state_h': nrm((nr, DEC_BATCH, D_RNN), 0.5),
        'ln_g': 1.0 + nrm((DEPTH, 2, D_MODEL), 0.02),
        'ln_b': nrm((DEPTH, 2, D_MODEL), 0.02),
        'w_attn_in': nrm((na, D_MODEL, 3 * D_MODEL), D_MODEL ** -0.5),
        'rel_bias': nrm((na, N_HEADS, 2 * REL_CLIP + 1), 0.1),
        'w_attn_out': nrm((na, D_MODEL, D_MODEL), BETA * D_MODEL ** -0.5),
        'w_rnn_in': nrm((nr, D_MODEL, 2 * D_RNN), D_MODEL ** -0.5),
        'conv_w': nrm((nr, CONV_W, D_RNN), CONV_W ** -0.5),
        'conv_b': nrm((nr, D_RNN), 0.02),
        'w_rg_a': nrm((nr, N_RNN_BLOCKS, RNN_BLOCK, RNN_BLOCK), RNN_BLOCK ** -0.5),
        'b_rg_a': nrm((nr, N_RNN_BLOCKS, RNN_BLOCK), 0.02),
        'w_rg_x': nrm((nr, N_RNN_BLOCKS, RNN_BLOCK, RNN_BLOCK), RNN_BLOCK ** -0.5),
        'b_rg_x': nrm((nr, N_RNN_BLOCKS, RNN_BLOCK), 0.02),
        'rg_lambda': rg_lambda,
        'w_rnn_out': nrm((nr, D_RNN, D_MODEL), BETA * D_RNN ** -0.5),
        'w_ffn_in': nrm((N_DENSE_FFN, D_MODEL, 2 * D_FF), D_MODEL ** -0.5),
        'w_ffn_out': nrm((N_DENSE_FFN, D_FF, D_MODEL), BETA * D_FF ** -0.5),
        'w_router': nrm((N_MOE_FFN, D_MODEL, N_EXPERTS), D_MODEL ** -0.5),
        'w_moe_in': nrm((N_MOE_FFN, N_EXPERTS, D_MODEL, 2 * MOE_FF), D_MODEL ** -0.5),
        'w_moe_out': nrm((N_MOE_FFN, N_EXPERTS, MOE_FF, D_MODEL), BETA * MOE_FF ** -0.5),
    }


def reference(x_prompt, x_sample, cache_k, cache_v, state_conv, state_h, ln_g, ln_b,
              w_attn_in, rel_bias, w_attn_out, w_rnn_in, conv_w, conv_b, w_rg_a, b_rg_a,
              w_rg_x, b_rg_x, rg_lambda, w_rnn_out, w_ffn_in, w_ffn_out, w_router, w_moe_in, w_moe_out):
    bp, sp, _ = x_prompt.shape
    bs, ts, _ = x_sample.shape
    keep = min(ATT_REACH, sp)
    xp, xs = x_prompt, x_sample
    kp_l, vp_l, ks_l, vs_l = [], [], [], []
    cp_l, hp_l, cs_l, hs_l = [], [], [], []
    for layer in range(DEPTH):
        j = layer // 2
        if layer % 2 == 0:
            qp, kp, vp = qkv_heads(xp, w_attn_in[j])
            qs, ks, vs = qkv_heads(xs, w_attn_in[j])
            op = chunk_attention_prompt(qp, kp, vp, rel_bias[j])
            o_s = chunk_attention_sample(qs, ks, vs, cache_k[j], cache_v[j], rel_bias[j])
            mp = op.reshape(bp, sp, D_MODEL) @ w_attn_out[j]
            ms = o_s.reshape(bs, ts, D_MODEL) @ w_attn_out[j]
            kp_l.append(kp[:, sp - keep:])
            vp_l.append(vp[:, sp - keep:])
            ks_l.append(ks)
            vs_l.append(vs)
        else:
            conv0 = jnp.zeros((bp, CONV_W - 1, D_RNN), xp.dtype)
            h_init = jnp.zeros((bp, D_RNN), xp.dtype)
            mp, cp, hp = rglru_mixer(xp, conv0, h_init, w_rnn_in[j], conv_w[j], conv_b[j], w_rg_a[j], b_rg_a[j],
                                     w_rg_x[j], b_rg_x[j], rg_lambda[j], w_rnn_out[j])
            ms, cs, hs = rglru_mixer(xs, state_conv[j], state_h[j], w_rnn_in[j], conv_w[j], conv_b[j], w_rg_a[j],
                                     b_rg_a[j], w_rg_x[j], b_rg_x[j], rg_lambda[j], w_rnn_out[j])
            cp_l.append(cp)
            hp_l.append(hp)
            cs_l.append(cs)
            hs_l.append(hs)
        xp = layer_norm(ALPHA * xp + mp, ln_g[layer, 0], ln_b[layer, 0])
        xs = layer_norm(ALPHA * xs + ms, ln_g[layer, 0], ln_b[layer, 0])
        flat = jnp.concatenate([xp.reshape(-1, D_MODEL), xs.reshape(-1, D_MODEL)], axis=0)
        if layer % 2 == 0:
            f = swiglu(flat, w_ffn_in[j], w_ffn_out[j])
        else:
            f = moe_swiglu(flat, w_router[j], w_moe_in[j], w_moe_out[j])
        fp = f[:bp * sp].reshape(bp, sp, D_MODEL)
        fs = f[bp * sp:].reshape(bs, ts, D_MODEL)
        xp = layer_norm(ALPHA * xp + fp, ln_g[layer, 1], ln_b[layer, 1])
        xs = layer_norm(ALPHA * xs + fs, ln_g[layer, 1], ln_b[layer, 1])
    y_prompt, y_sample = xp, xs
    return (y_prompt, y_sample, jnp.stack(kp_l), jnp.stack(vp_l), jnp.stack(ks_l), jnp.stack(vs_l),
            jnp.stack(cp_l), jnp.stack(hp_l), jnp.stack(cs_l), jnp.stack(hs_l))
```

```python
import os
from contextlib import ExitStack
import numpy as np
import concourse.bass as bass
import concourse.mybir as mybir
from concourse.bass_utils import run_bass_kernel_spmd

F32 = mybir.dt.float32
BF16 = mybir.dt.bfloat16
AF = mybir.ActivationFunctionType
ALU = mybir.AluOpType

D = 2048
KC = 16
NH = 32
HD = 64
DFF = 5632
NEXP = 8
ALPHA = (2.0 * 2) ** 0.25
LN_EPS = 1e-5
NEG = -30000.0
VS = 66
ENGS = ("sync", "scalar", "vector", "gpsimd", "tensor")
DEBUG = bool(int(os.environ.get("MK_DEBUG", "0")))
ATT_STAGE = int(os.environ.get("MK_ATT_STAGE", "3"))
NOXCH = bool(int(os.environ.get("MK_NOXCH", "0")))


class Sem:
    def __init__(self, h, step):
        self.h, self.step, self.n = h, step, 0

    def next(self):
        self.n += self.step
        return (self.h, self.n)


def _flat(ws):
    out = []
    for w in ws:
        if w is None:
            continue
        if isinstance(w, list):
            out.extend(_flat(w))
        else:
            out.append(w)
    return out


class Op:
    __slots__ = ("waits", "build", "sig")

    def __init__(self, waits, build, sig):
        self.waits, self.build, self.sig = waits, build, sig


class GlobalSems:
    def __init__(self, nc):
        self.es = ExitStack()
        self.es.__enter__()
        mk = lambda nm: self.es.enter_context(nc.semaphore(nm))
        self.esem = {e: Sem(mk("e_" + e), 1) for e in ("scalar", "vector", "gpsimd", "tensor")}
        self.cc = Sem(mk("cc"), 16)
        self.pool = {"sync": [Sem(mk(f"ds{i}"), 16) for i in range(20)],
                     "gpsimd": [Sem(mk(f"dp{i}"), 16) for i in range(10)]}


class LazySem:
    def __init__(self):
        self.s = None


_G = {}


class Phase:
    def __init__(self, nc, name):
        self.nc, self.name = nc, name
        self.es = ExitStack()
        self.q = {e: [] for e in ENGS}
        self.store_toks = []
        if id(nc) not in _G:
            _G.clear()
            _G[id(nc)] = GlobalSems(nc)
        self.G = _G[id(nc)]
        self.esem = self.G.esem
        self.used = {"sync": 0, "gpsimd": 0}

    def __enter__(self):
        self.es.__enter__()
        return self

    def __exit__(self, *a):
        return self.es.__exit__(*a)

    def dsem(self, name=None):
        return LazySem()

    def bind(self, ls, eng):
        if ls.s is None:
            q = "gpsimd" if eng == "gpsimd" else "sync"
            ls.s = self.G.pool[q][self.used[q]]
            self.used[q] += 1
        return ls.s

    def sb(self, name, shape, dt):
        return self.es.enter_context(self.nc.sbuf_tensor(f"{self.name}_{name}", list(shape), dt))

    def ps(self, name, shape, dt=F32):
        return self.es.enter_context(self.nc.psum_tensor(f"{self.name}_{name}", list(shape), dt))

    def op(self, eng, build, waits=(), sig=False):
        o = Op(_flat(waits), build, None)
        tok = None
        if sig:
            s = self.esem[eng]
            o.sig = s
            tok = s.next()
        self.q[eng].append(o)
        return tok

    def dma(self, eng, out, in_, dsem, waits=(), store=False):
        dsem = self.bind(dsem, eng)
        def build(e):
            try:
                return e.dma_start(out=out, in_=in_)
            except ValueError:
                return e.dma_start(out=out, in_=in_, allow_slow_non_contiguous=True)

        o = Op(_flat(waits), build, dsem)
        tok = dsem.next()
        self.q[eng].append(o)
        if store:
            self.store_toks.append(tok)
        return tok

    def sig_last(self, eng):
        q = self.q[eng]
        s = self.esem[eng]
        for o in reversed(q):
            if o.build is not None:
                if o.sig is None:
                    o.sig = s
                    return s.next()
                if o.sig is s:
                    return (s.h, s.n)
                break
        return None

    def flush(self):
        fin = []
        for e in ("scalar", "vector", "gpsimd", "tensor"):
            t = self.sig_last(e)
            if t is not None:
                fin.append(t)
        st = {}
        for (h, v) in self.store_toks:
            k = id(h)
            if k not in st or st[k][1] < v:
                st[k] = (h, v)
        fin = fin + list(st.values())
        for e in ENGS:
            self.q[e].append(Op(list(fin), None, None))
        nc = self.nc
        with nc.Block() as blk:
            for e in ENGS:
                ops = self.q[e]

                def run(eng, ops=ops):
                    seen = {}
                    for o in ops:
                        for (h, v) in o.waits:
                            k = id(h)
                            if seen.get(k, -1) >= v:
                                continue
                            seen[k] = v
                            eng.wait_ge(h, v)
                        if o.build is None:
                            continue
                        ins = o.build(eng)
                        if o.sig is not None:
                            ins.then_inc(o.sig.h, o.sig.step)

                getattr(blk, e)(run)


def _evac(P, i, out, in_, waits, scale=None):
    if i % 2 == 0:
        if scale is None:
            return P.op("vector", lambda e: e.tensor_copy(out=out, in_=in_), waits=waits, sig=True)
        return P.op("vector", lambda e: e.tensor_scalar(out=out, in0=in_, scalar1=float(scale), scalar2=None,
                                                        op0=ALU.mult), waits=waits, sig=True)
    return P.op("scalar", lambda e: e.activation(out=out, in_=in_, func=AF.Copy,
                                                 scale=(1.0 if scale is None else float(scale))),
                waits=waits, sig=True)


def transpose_phase(nc, name, ident_in, jobs, KCk, out_dt=BF16, scale=None, prep=None, prep_setup=None, dst3=False):
    with Phase(nc, name) as P:
        ident = P.sb("ident", [128, 128], F32)
        xt = [P.sb(f"x{i}", [128, KCk * 128], F32) for i in range(2)]
        xT = [P.sb(f"xT{i}", [128, KCk * 128], out_dt) for i in range(2)]
        pst = [P.ps(f"ps{i}", [128, 512], F32) for i in range(4)]
        s_id = P.dsem("id")
        s_x = [P.dsem(f"x{i}") for i in range(2)]
        s_o = [P.dsem(f"o{i}") for i in range(2)]
        t_id = P.dma("sync", ident[:], ident_in[:, :], s_id)
        st = prep_setup(P) if prep_setup is not None else None
        x_free = [None, None]
        xT_free = [None, None]
        ps_free = [None] * 4
        NG = KCk // 4
        gi = 0
        for t, (src, dst) in enumerate(jobs):
            b = t % 2
            if prep is None:
                t_ld = P.dma("sync", xt[b][:], src, s_x[b], waits=[x_free[b]])
            else:
                t_ld = prep(P, st, t, xt[b], x_free[b])
            ev = []
            tk = None
            for g in range(NG):
                pb = gi % 4
                gi += 1
                for j in range(4):
                    kc = g * 4 + j
                    tk = P.op("tensor", lambda e, pb=pb, j=j, kc=kc, b=b: e.transpose(
                        pst[pb][:, j * 128:(j + 1) * 128], xt[b][:, kc * 128:(kc + 1) * 128], ident[:]),
                        waits=[t_ld, t_id, ps_free[pb]] if j == 0 else [], sig=(j == 3))
                te = _evac(P, g, xT[b][:, g * 512:(g + 1) * 512], pst[pb][:], [tk, xT_free[b]], scale)
                ps_free[pb] = te
                ev.append(te)
            x_free[b] = tk
            srcv = xT[b][:].rearrange("p (k t) -> p k t", t=128) if dst3 else xT[b][:]
            xT_free[b] = P.dma("gpsimd", dst, srcv, s_o[b], waits=ev, store=True)
        P.flush()


def gelu_prep_setup(P):
    return dict(g=[P.sb(f"gg{i}", [128, D], F32) for i in range(2)], t=[P.sb(f"gt{i}", [128, D], F32) for i in range(2)],
                s=[P.dsem(f"gs{i}") for i in range(2)], free=[None, None])


def make_gelu_prep(srcs):
    def prep(P, st, t, dst_tile, dst_free):
        b = t % 2
        g, tt = st["g"][b], st["t"][b]
        t_l = P.dma("sync", g[:], srcs[t], st["s"][b], waits=[st["free"][b]])
        t_1 = P.op("scalar", lambda e: e.activation(out=tt[:], in_=g[:], func=AF.Square), waits=[t_l, st["free"][b]], sig=True)
        t_2 = P.op("vector", lambda e: e.tensor_scalar(out=tt[:], in0=tt[:], scalar1=0.044715, scalar2=1.0,
                                                       op0=ALU.mult, op1=ALU.add), waits=[t_1], sig=True)
        t_3 = P.op("vector", lambda e: e.tensor_tensor(out=tt[:], in0=tt[:], in1=g[:], op=ALU.mult), waits=[t_2], sig=True)
        t_4 = P.op("scalar", lambda e: e.activation(out=tt[:], in_=tt[:], func=AF.Sigmoid, scale=1.5957691216057308),
                   waits=[t_3], sig=True)
        t_5 = P.op("vector", lambda e: e.tensor_tensor(out=dst_tile[:], in0=tt[:], in1=g[:], op=ALU.mult),
                   waits=[t_4, dst_free], sig=True)
        st["free"][b] = t_5
        return t_5
    return prep


class AccEpi:
    def __init__(self, FA, GATES, e):
        self.FA, self.G, self.e = FA, GATES, e

    def setup(self, P):
        self.acc = [P.sb(f"acc{i}", [128, 512], F32) for i in range(3)]
        self.gt = [P.sb(f"gt{i}", [128, 8], F32) for i in range(3)]
        self.sa = [P.dsem(f"sa{i}") for i in range(3)]
        self.sg = [P.dsem(f"sg{i}") for i in range(3)]
        self.so = [P.dsem(f"so{i}") for i in range(3)]
        self.free = [None] * 3
        self.i = 0

    def __call__(self, P, tile, blk, pss, ready):
        k = self.i % 3
        self.i += 1
        e = self.e
        dst = self.FA[tile * 128:(tile + 1) * 128, blk * 512:(blk + 1) * 512]
        t_g = P.dma("sync", self.gt[k][:], self.G[tile * 128:(tile + 1) * 128, :], self.sg[k], waits=[self.free[k]])
        if e == 0:
            t_o = P.op("vector", lambda en: en.tensor_scalar(out=self.acc[k][:], in0=pss[0][:], scalar1=self.gt[k][:, e:e + 1],
                                                             scalar2=None, op0=ALU.mult), waits=[ready, t_g, self.free[k]], sig=True)
        else:
            t_a = P.dma("sync", self.acc[k][:], dst, self.sa[k], waits=[self.free[k]])
            t_o = P.op("vector", lambda en: en.scalar_tensor_tensor(out=self.acc[k][:], in0=pss[0][:], scalar=self.gt[k][:, e:e + 1],
                                                                    in1=self.acc[k][:], op0=ALU.mult, op1=ALU.add),
                       waits=[ready, t_g, t_a], sig=True)
        self.free[k] = P.dma("gpsimd", dst, self.acc[k][:], self.so[k], waits=[t_o], store=True)
        return [t_o]


class StoreEpi:
    def __init__(self, Y, col_of_block):
        self.Y, self.col_of_block = Y, col_of_block

    def setup(self, P):
        self.ob = [P.sb(f"ob{i}", [128, 512], F32) for i in range(4)]
        self.so = [P.dsem(f"so{i}") for i in range(4)]
        self.free = [None] * 4
        self.i = 0

    def __call__(self, P, tile, blk, pss, ready):
        done = []
        for s, ps in enumerate(pss):
            k = self.i % 4
            self.i += 1
            te = _evac(P, self.i, self.ob[k][:], ps[:], [ready, self.free[k]])
            c0 = self.col_of_block(blk, s)
            self.free[k] = P.dma("gpsimd", self.Y[tile * 128:(tile + 1) * 128, c0:c0 + 512], self.ob[k][:], self.so[k],
                                 waits=[te], store=True)
            done.append(te)
        return done


def gemm_phase(nc, name, XT, tiles, KCk, W, blocks, epi):
    nsub = len(blocks[0])
    with Phase(nc, name) as P:
        wst = [P.sb(f"wst{i}", [128, 4, 512], F32) for i in range(3)]
        wb = [P.sb(f"wb{i}", [128, nsub * KCk, 512], BF16) for i in range(2)]
        xb = [P.sb(f"xb{i}", [128, KCk * 128], BF16) for i in range(3)]
        NPS = 4 if nsub == 1 else 3
        ps = [[P.ps(f"ps{i}_{s}", [128, 512], F32) for s in range(nsub)] for i in range(NPS)]
        s_w = [P.dsem(f"w{i}") for i in range(3)]
        s_x = [P.dsem(f"x{i}") for i in range(3)]
        epi.setup(P)
        wst_free = [None] * 3
        wb_free = [None] * 2
        xb_free = [None] * 3
        ps_free = [None] * NPS
        wi = 0
        xi = 0
        pi = 0
        for bi, cols in enumerate(blocks):
            wbuf = bi % 2
            cast_toks = []
            for s, c0 in enumerate(cols):
                for k0 in range(0, KCk, 4):
                    k = wi % 3
                    wi += 1
                    src = W[k0 * 128:(k0 + 4) * 128, c0:c0 + 512].rearrange("(k p) n -> p k n", p=128)
                    t_ld = P.dma("sync", wst[k][:], src, s_w[k], waits=[wst_free[k]])
                    dst = wb[wbuf][:, s * KCk + k0:s * KCk + k0 + 4, :]
                    eng = ("vector", "gpsimd", "scalar")[wi % 3]
                    if eng == "scalar":
                        tc_ = P.op("scalar", lambda e, dst=dst, k=k: e.activation(out=dst, in_=wst[k][:], func=AF.Copy),
                                   waits=[t_ld, wb_free[wbuf]], sig=True)
                    else:
                        tc_ = P.op(eng, lambda e, dst=dst, k=k: e.tensor_copy(out=dst, in_=wst[k][:]),
                                   waits=[t_ld, wb_free[wbuf]], sig=True)
                    wst_free[k] = tc_
                    cast_toks.append(tc_)
            last_mm = None
            for tile in tiles:
                xk = xi % 3
                xi += 1
                t_x = P.dma("sync", xb[xk][:], XT[tile], s_x[xk], waits=[xb_free[xk]])
                pk = pi % NPS
                pi += 1
                for s in range(nsub):
                    for kc in range(KCk):
                        last_mm = P.op("tensor", lambda e, pk=pk, s=s, kc=kc, xk=xk, wbuf=wbuf: e.matmul(
                            ps[pk][s][:], xb[xk][:, kc * 128:(kc + 1) * 128], wb[wbuf][:, s * KCk + kc, :],
                            start=(kc == 0), stop=(kc == KCk - 1)),
                            waits=([t_x, ps_free[pk]] + cast_toks) if (s == 0 and kc == 0) else [],
                            sig=(s == nsub - 1 and kc == KCk - 1))
                xb_free[xk] = last_mm
                done = epi(P, tile, bi, ps[pk], last_mm)
                ps_free[pk] = done[-1] if len(done) == 1 else None
                if len(done) > 1:
                    ps_free[pk] = done
            wb_free[wbuf] = last_mm
        P.flush()


def ln_phase(nc, name, jobs, g_row, b_row, hook=None, hook_setup=None):
    with Phase(nc, name) as P:
        gt = P.sb("g", [128, D], F32)
        bt = P.sb("b", [128, D], F32)
        xa = [P.sb(f"xa{i}", [128, D], F32) for i in range(2)]
        ma = [P.sb(f"ma{i}", [128, D], F32) for i in range(2)]
        ya = [P.sb(f"ya{i}", [128, D], F32) for i in range(2)]
        st = [P.sb(f"st{i}", [128, 4, 6], F32) for i in range(2)]
        mv = [P.sb(f"mv{i}", [128, 4], F32) for i in range(2)]
        s_c = P.dsem("c")
        s_x = [P.dsem(f"x{i}") for i in range(2)]
        s_m = [P.dsem(f"m{i}") for i in range(2)]
        s_o = [P.dsem(f"o{i}") for i in range(2)]
        P.dma("sync", gt[:], g_row.to_broadcast([128, D]), s_c)
        t_c = P.dma("sync", bt[:], b_row.to_broadcast([128, D]), s_c)
        hs = hook_setup(P) if hook_setup is not None else None
        x_free = [None, None]
        y_free = [None, None]
        for t, (xs, ms, dsts) in enumerate(jobs):
            b = t % 2
            t_x = P.dma("sync", xa[b][:], xs, s_x[b], waits=[x_free[b]])
            t_m = P.dma("sync", ma[b][:], ms, s_m[b], waits=[x_free[b]])
            t_z = P.op("vector", lambda e, b=b: e.scalar_tensor_tensor(out=ma[b][:], in0=xa[b][:], scalar=float(ALPHA),
                                                                       in1=ma[b][:], op0=ALU.mult, op1=ALU.add),
                       waits=[t_x, t_m], sig=True)
            t_s = []
            for j in range(4):
                t_s.append(P.op("vector", lambda e, b=b, j=j: e.bn_stats(out=st[b][:, j, :], in_=ma[b][:, j * 512:(j + 1) * 512]),
                                waits=[t_z] if j == 0 else [], sig=(j == 3)))
            t_a = P.op("vector", lambda e, b=b: e.bn_aggr(out=mv[b][:, 0:2], in_=st[b][:].rearrange("p a b -> p (a b)")),
                       waits=[t_s[-1]], sig=True)
            t_v = P.op("vector", lambda e, b=b: e.tensor_scalar(out=mv[b][:, 2:3], in0=mv[b][:, 1:2], scalar1=float(LN_EPS),
                                                                scalar2=None, op0=ALU.add), waits=[t_a], sig=True)
            t_sq = P.op("scalar", lambda e, b=b: e.activation(out=mv[b][:, 2:3], in_=mv[b][:, 2:3], func=AF.Sqrt),
                        waits=[t_v], sig=True)
            t_r = P.op("vector", lambda e, b=b: e.reciprocal(out=mv[b][:, 3:4], in_=mv[b][:, 2:3]), waits=[t_sq], sig=True)
            t_n0 = P.op("vector", lambda e, b=b: e.tensor_scalar(out=xa[b][:], in0=ma[b][:], scalar1=mv[b][:, 0:1],
                                                             scalar2=mv[b][:, 3:4], op0=ALU.subtract, op1=ALU.mult),
                        waits=[t_r], sig=True)
            t_n = P.op("vector", lambda e, b=b: e.tensor_tensor(out=xa[b][:], in0=xa[b][:], in1=gt[:], op=ALU.mult),
                       waits=[t_c, t_n0], sig=True)
            t_y = P.op("gpsimd", lambda e, b=b: e.tensor_tensor(out=ya[b][:], in0=xa[b][:], in1=bt[:], op=ALU.add),
                       waits=[t_n, y_free[b], t_c], sig=True)
            x_free[b] = t_y
            toks = []
            for (dst, nrows) in dsts:
                toks.append(P.dma("gpsimd", dst, ya[b][0:nrows, :], s_o[b], waits=[t_y], store=True))
            if hook is not None:
                toks.append(hook(P, hs, t, ya[b], t_y))
            y_free[b] = toks
        P.flush()


def v1_phase(nc, name, jobs, flags_in):
    with Phase(nc, name) as P:
        fl = P.sb("fl", [128, 16], F32)
        xa = [P.sb(f"xa{i}", [128, D], F32) for i in range(2)]
        va = [P.sb(f"va{i}", [128, NH * VS], BF16) for i in range(2)]
        s_c = P.dsem("c")
        s_x = [P.dsem(f"x{i}") for i in range(2)]
        s_o = [P.dsem(f"o{i}") for i in range(2)]
        t_c = P.dma("sync", fl[:], flags_in[:, :], s_c)
        x_free = [None, None]
        v_free = [None, None]
        for t, (src, dst, fcol) in enumerate(jobs):
            b = t % 2
            t_x = P.dma("sync", xa[b][:], src, s_x[b], waits=[x_free[b]])
            v3 = va[b][:].rearrange("p (h c) -> p h c", c=VS)
            t_0 = P.op("gpsimd", lambda e, v3=v3: e.memset(v3[:, :, 65:VS], 0.0), waits=[v_free[b]], sig=True)
            t_1 = P.op("vector", lambda e, b=b, v3=v3: e.tensor_copy(out=v3[:, :, 0:64],
                                                                    in_=xa[b][:].rearrange("p (h c) -> p h c", c=64)),
                       waits=[t_x, v_free[b]], sig=True)
            if fcol is None:
                t_2 = P.op("gpsimd", lambda e, v3=v3: e.memset(v3[:, :, 64:65], 1.0), waits=[v_free[b], t_0], sig=True)
            else:
                t_2 = P.op("gpsimd", lambda e, v3=v3, fcol=fcol: e.tensor_copy(
                    out=v3[:, :, 64:65], in_=fl[:, fcol:fcol + 1].unsqueeze(1).to_broadcast([128, NH, 1])),
                    waits=[v_free[b], t_c, t_0], sig=True)
            x_free[b] = t_1
            v_free[b] = P.dma("gpsimd", dst, va[b][:], s_o[b], waits=[t_1, t_2], store=True)
        P.flush()


def attn_phase(nc, name, units, ident_in, bt_in, QT, KT, V1, OA):
    with Phase(nc, name) as P:
        identf = P.sb("identf", [128, 128], F32)
        identb = P.sb("identb", [128, 128], BF16)
        btf = P.sb("btf", [128, 1024], F32)
        btb = P.sb("btb", [128, 5, NH * 128], BF16)
        qe = [P.sb(f"qe{i}", [128, KC * 128], BF16) for i in range(2)]
        qo = [P.sb(f"qo{i}", [128, KC * 128], BF16) for i in range(2)]
        kt = [P.sb(f"kt{i}", [128, 5, KC * 128], BF16) for i in range(2)]
        v1 = [P.sb(f"v1{i}", [128, 5, NH * VS], BF16) for i in range(2)]
        et = [P.sb(f"et{i}", [128, 5, 512], BF16) for i in range(2)]
        osb = [P.sb(f"osb{i}", [128, NH * 65], F32) for i in range(2)]
        rc = [P.sb(f"rc{i}", [128, NH], F32) for i in range(2)]
        oa = [P.sb(f"oa{i}", [128, D], F32) for i in range(2)]
        pS = [P.ps(f"pS{i}", [128, 512], F32) for i in range(3)]
        pO = [P.ps(f"pO{i}", [128, 512], F32) for i in range(2)]
        s_c = P.dsem("c")
        s_bt = P.dsem("bt")
        s_q = [P.dsem(f"q{i}") for i in range(2)]
        s_k = [P.dsem(f"k{i}") for i in range(2)]
        s_v = [P.dsem(f"v{i}") for i in range(2)]
        s_o = [P.dsem(f"o{i}") for i in range(2)]
        t_id = P.dma("sync", identf[:], ident_in[:, :], s_c)
        t_idb = P.op("vector", lambda e: e.tensor_copy(out=identb[:], in_=identf[:]), waits=[t_id], sig=True)
        t_z = None
        for i in range(2):
            P.op("gpsimd", lambda e, i=i: e.memset(qe[i][64:128, :], 0.0))
            t_z = P.op("gpsimd", lambda e, i=i: e.memset(qo[i][0:64, :], 0.0), sig=True)
        t_bt = None
        for kb in range(5):
            for c in range(4):
                t_l = P.dma("sync", btf[:], bt_in[kb, :, c * 1024:(c + 1) * 1024], s_bt, waits=[t_bt])
                t_bt = P.op("vector", lambda e, kb=kb, c=c: e.tensor_copy(out=btb[:, kb, c * 1024:(c + 1) * 1024], in_=btf[:]),
                            waits=[t_l], sig=True)
        q_free = [None, None]
        k_free = [None, None]
        v_free = [None, None]
        et_free = [None, None]
        osb_free = [None, None]
        oa_free = [None, None]
        pS_free = [None] * 3
        pO_free = [None] * 2
        si = 0
        gi = 0
        for u, (tq, ktiles) in enumerate(units):
            b = u % 2
            P.dma("sync", qe[b][0:64, :], QT[tq][0:64, :], s_q[b], waits=[q_free[b], t_z])
            t_q = P.dma("sync", qo[b][64:128, :], QT[tq][64:128, :], s_q[b], waits=[q_free[b], t_z])
            t_k = None
            t_v = None
            for j, tk_ in enumerate(ktiles):
                t_k = P.dma("sync", kt[b][:, j, :], KT[tk_], s_k[b], waits=[k_free[b]] if j == 0 else [])
                t_v = P.dma("sync", v1[b][:, j, :], V1[tk_], s_v[b], waits=[v_free[b]] if j == 0 else [])
            last_pv = None
            ev_toks = []
            for hg in range(8):
                eb = gi % 2
                pob = gi % 2
                gi += 1
                exp_toks = []
                for kb in range(5):
                    sb_ = si % 3
                    si += 1
                    P.op("tensor", lambda e, sb_=sb_, kb=kb, hg=hg: e.matmul(
                        pS[sb_][:], identb[:], btb[:, kb, hg * 512:(hg + 1) * 512], start=True, stop=False),
                        waits=[pS_free[sb_], t_bt, t_idb, t_q, t_k])
                    for hh in range(4):
                        h = hg * 4 + hh
                        kc, p0 = h // 2, (h % 2) * 64
                        qsel = qe if p0 == 0 else qo
                        t_s = P.op("tensor", lambda e, sb_=sb_, kb=kb, hh=hh, kc=kc, qsel=qsel, b=b: e.matmul(
                            pS[sb_][:, hh * 128:(hh + 1) * 128],
                            kt[b][:, kb, kc * 128:(kc + 1) * 128],
                            qsel[b][:, kc * 128:(kc + 1) * 128], start=False, stop=(hh == 3)),
                            sig=(hh == 3))
                    t_e = P.op("scalar", lambda e, sb_=sb_, kb=kb, eb=eb: e.activation(
                        out=et[eb][:, kb, :], in_=pS[sb_][:], func=AF.Exp),
                        waits=[t_s, et_free[eb]] if kb == 0 else [t_s], sig=True)
                    pS_free[sb_] = t_e
                    exp_toks.append(t_e)
                for hh in range(4):
                    h = hg * 4 + hh
                    for kb in range(5):
                        if ATT_STAGE < 2 and not (hh == 3 and kb == 4):
                            continue
                        last_pv = P.op("tensor", lambda e, pob=pob, hh=hh, kb=kb, h=h, eb=eb, b=b: e.matmul(
                            pO[pob][:, hh * 65:(hh + 1) * 65], et[eb][:, kb, hh * 128:(hh + 1) * 128],
                            v1[b][:, kb, h * VS:h * VS + 65], start=(kb == 0), stop=(kb == 4)),
                            waits=(exp_toks + [pO_free[pob], t_v]) if (hh == 0 and kb == 0) else [],
                            sig=(hh == 3 and kb == 4))
                et_free[eb] = last_pv
                t_ev = P.op("vector", lambda e, pob=pob, hg=hg, b=b: e.tensor_copy(
                    out=osb[b][:, hg * 260:(hg + 1) * 260], in_=pO[pob][:, 0:260]),
                    waits=[last_pv, osb_free[b]] if hg == 0 else [last_pv], sig=True)
                pO_free[pob] = t_ev
                ev_toks.append(t_ev)
            q_free[b] = last_pv
            k_free[b] = last_pv
            v_free[b] = last_pv
            o3 = osb[b][:].rearrange("p (h c) -> p h c", c=65)
            if ATT_STAGE < 3:
                t_n = P.op("vector", lambda e, b=b: e.tensor_copy(out=oa[b][:], in_=osb[b][:, 0:D]),
                           waits=ev_toks + [oa_free[b]], sig=True)
                osb_free[b] = t_n
                oa_free[b] = P.dma("gpsimd", OA[tq * 128:(tq + 1) * 128, :], oa[b][:], s_o[b], waits=[t_n], store=True)
                continue
            t_r0 = P.op("vector", lambda e, b=b, o3=o3: e.tensor_scalar(out=rc[b][:].unsqueeze(2), in0=o3[:, :, 64:65], scalar1=1e-30,
                                                                        scalar2=None, op0=ALU.max), waits=ev_toks, sig=True)
            t_r = P.op("vector", lambda e, b=b: e.reciprocal(out=rc[b][:], in_=rc[b][:]), waits=[t_r0], sig=True)
            t_n = P.op("vector", lambda e, b=b, o3=o3: e.tensor_tensor(
                out=oa[b][:].rearrange("p (h c) -> p h c", c=64), in0=o3[:, :, 0:64],
                in1=rc[b][:].unsqueeze(2).to_broadcast([128, NH, 64]), op=ALU.mult),
                waits=[t_r, oa_free[b]], sig=True)
            osb_free[b] = t_n
            oa_free[b] = P.dma("gpsimd", OA[tq * 128:(tq + 1) * 128, :], oa[b][:], s_o[b], waits=[t_n], store=True)
        P.flush()


class SwiGLUEpi:
    def __init__(self, H):
        self.H = H

    def setup(self, P):
        self.sg = [P.sb(f"sg{i}", [128, 512], F32) for i in range(3)]
        self.ob = [P.sb(f"ob{i}", [128, 512], F32) for i in range(3)]
        self.so = [P.dsem(f"so{i}") for i in range(3)]
        self.free = [None] * 3
        self.sgfree = [None] * 3
        self.i = 0

    def __call__(self, P, tile, blk, pss, ready):
        k = self.i % 3
        self.i += 1
        t_s = P.op("scalar", lambda e: e.activation(out=self.sg[k][:], in_=pss[0][:], func=AF.Silu),
                   waits=[ready, self.sgfree[k]], sig=True)
        t_h = P.op("vector", lambda e: e.tensor_tensor(out=self.ob[k][:], in0=self.sg[k][:], in1=pss[1][:], op=ALU.mult),
                   waits=[t_s, self.free[k]], sig=True)
        self.sgfree[k] = t_h
        self.free[k] = P.dma("gpsimd", self.H[tile * 128:(tile + 1) * 128, blk * 512:(blk + 1) * 512], self.ob[k][:],
                             self.so[k], waits=[t_h], store=True)
        return [t_h]


def copy_phase(nc, name, pairs):
    with Phase(nc, name) as P:
        s = P.dsem("c")
        for (dst, src) in pairs:
            P.dma("sync", dst, src, s, store=True)
        P.flush()


def scan_phase(nc, name, seqs, UT, GGT, XTout, STd, pf_in, w_rg_a, w_rg_x, emit):
    SEG = 512
    with Phase(nc, name) as P:
        pf = P.sb("pf", [128, 8, KC], F32)
        cl = P.sb("cl", [128, KC], F32)
        wst = P.sb("wst", [128, 2, 256], F32)
        wa = [P.sb(f"wa{i}", [128, 2, 256], BF16) for i in range(2)]
        wx = [P.sb(f"wx{i}", [128, 2, 256], BF16) for i in range(2)]
        ub = [P.sb(f"ub{i}", [128, 2, 3 + SEG], F32) for i in range(2)]
        gb = [P.sb(f"gb{i}", [128, 2, SEG], F32) for i in range(2)]
        uc = [P.sb(f"uc{i}", [128, 2, SEG], F32) for i in range(2)]
        ucb = [P.sb(f"ucb{i}", [128, 2, SEG], BF16) for i in range(2)]
        rr = P.sb("rr", [128, 2, SEG], F32)
        ii = P.sb("ii", [128, 2, SEG], F32)
        aa = P.sb("aa", [128, 2, SEG], F32)
        ss = P.sb("ss", [128, 2, SEG], F32)
        hh = P.sb("hh", [128, 2, SEG], F32)
        yb = [P.sb(f"yb{i}", [128, 2, SEG], BF16) for i in range(2)]
        carry = P.sb("carry", [128, 2], F32)
        stt = [P.sb(f"stt{i}", [128, 2, 4], F32) for i in range(2)]
        pr = [P.ps(f"pr{i}", [128, SEG], F32) for i in range(4)]
        s_c = P.dsem("c")
        s_w = P.dsem("w")
        s_u = [P.dsem(f"u{i}") for i in range(2)]
        s_g = [P.dsem(f"g{i}") for i in range(2)]
        s_h = P.dsem("h")
        s_y = [[P.dsem(), P.dsem()] for i in range(2)]
        s_s = [P.dsem(f"s{i}") for i in range(2)]
        t_pf = P.dma("sync", pf[:], pf_in[:, :, :], s_c)
        t0 = P.op("scalar", lambda e: e.activation(out=cl[:], in_=pf[:, 7, :], func=AF.Exp, scale=-1.0), waits=[t_pf], sig=True)
        t1 = P.op("scalar", lambda e: e.activation(out=cl[:], in_=cl[:], func=AF.Ln, bias=1.0), waits=[t0], sig=True)
        t_cl = P.op("vector", lambda e: e.tensor_scalar(out=cl[:], in0=cl[:], scalar1=-8.0, scalar2=None, op0=ALU.mult),
                    waits=[t1], sig=True)
        wst_free = None
        w_free = [None, None]
        u_free = [None, None]
        g_free = [None, None]
        y_free = [None, None]
        st_free = [None, None]
        pr_free = [None] * 4
        tail = None
        it = 0
        sti = 0
        for n in range(8):
            wbuf = n % 2
            t_l = P.dma("sync", wst[:], w_rg_a[n].rearrange("(k p) c -> p k c", p=128), s_w, waits=[wst_free])
            t_wa = P.op("vector", lambda e, wbuf=wbuf: e.tensor_copy(out=wa[wbuf][:], in_=wst[:]), waits=[t_l, w_free[wbuf]], sig=True)
            t_l = P.dma("sync", wst[:], w_rg_x[n].rearrange("(k p) c -> p k c", p=128), s_w, waits=[t_wa])
            t_wx = P.op("vector", lambda e, wbuf=wbuf: e.tensor_copy(out=wx[wbuf][:], in_=wst[:]), waits=[t_l, w_free[wbuf]], sig=True)
            wst_free = t_wx
            last_mm = None
            for sq in seqs:
                L_tot = sq["L"]
                t_h0 = P.dma("sync", carry[:].unsqueeze(2), sq["h0"][:, 2 * n:2 * n + 2, :], s_h, waits=[tail])
                nseg = (L_tot + SEG - 1) // SEG
                for sg in range(nseg):
                    b = it % 2
                    it += 1
                    c0 = sq["col0"] + sg * SEG
                    L = min(SEG, L_tot - sg * SEG)
                    halo = sq["halo"][:, 2 * n:2 * n + 2, :] if sg == 0 else UT[:, 2 * n:2 * n + 2, c0 - 3:c0]
                    t_u1 = P.dma("sync", ub[b][:, :, 0:3], halo, s_u[b], waits=[u_free[b]])
                    t_u = P.dma("sync", ub[b][:, :, 3:3 + L], UT[:, 2 * n:2 * n + 2, c0:c0 + L], s_u[b], waits=[u_free[b]])
                    if emit:
                        t_g = P.dma("sync", gb[b][:, :, 0:L], GGT[:, 2 * n:2 * n + 2, c0:c0 + L], s_g[b], waits=[g_free[b]])
                    tc = None
                    for j in range(2):
                        kc = 2 * n + j
                        tc = P.op("vector", lambda e, b=b, j=j, kc=kc, L=L: e.tensor_scalar(
                            out=uc[b][:, j, 0:L], in0=ub[b][:, j, 0:L], scalar1=pf[:, 0, kc:kc + 1], scalar2=pf[:, 4, kc:kc + 1],
                            op0=ALU.mult, op1=ALU.add), waits=[t_u, t_pf, tc, tail] if j == 0 else [tc], sig=True)
                        for i in range(1, 4):
                            tc = P.op("vector", lambda e, b=b, j=j, kc=kc, L=L, i=i: e.scalar_tensor_tensor(
                                out=uc[b][:, j, 0:L], in0=ub[b][:, j, i:i + L], scalar=pf[:, i, kc:kc + 1], in1=uc[b][:, j, 0:L],
                                op0=ALU.mult, op1=ALU.add), waits=[tc], sig=True)
                    t_cb = P.op("vector", lambda e, b=b, L=L: e.tensor_copy(out=ucb[b][:, :, 0:L], in_=uc[b][:, :, 0:L]),
                                waits=[tc, last_mm], sig=True)
                    gate_toks = []
                    for gsel, (wt, dstt, brow) in enumerate(((wa, rr, 5), (wx, ii, 6))):
                        for j in range(2):
                            pk = gsel * 2 + j
                            for ci in range(2):
                                last_mm = P.op("tensor", lambda e, pk=pk, wt=wt, wbuf=wbuf, ci=ci, j=j, b=b, L=L: e.matmul(
                                    pr[pk][:, 0:L], wt[wbuf][:, ci, j * 128:(j + 1) * 128], ucb[b][:, ci, 0:L],
                                    start=(ci == 0), stop=(ci == 1)),
                                    waits=[t_cb, t_wa, t_wx, pr_free[pk]] if ci == 0 else [], sig=(ci == 1))
                            kc = 2 * n + j
                            tg_ = P.op("scalar", lambda e, pk=pk, dstt=dstt, j=j, kc=kc, brow=brow, L=L: e.activation(
                                out=dstt[:, j, 0:L], in_=pr[pk][:, 0:L], func=AF.Sigmoid, bias=pf[:, brow, kc:kc + 1]),
                                waits=[last_mm, tail], sig=True)
                            pr_free[pk] = tg_
                            gate_toks.append(tg_)
                    ta = None
                    for j in range(2):
                        kc = 2 * n + j
                        ta = P.op("scalar", lambda e, j=j, kc=kc, L=L: e.activation(
                            out=aa[:, j, 0:L], in_=rr[:, j, 0:L], func=AF.Exp, scale=cl[:, kc:kc + 1]),
                            waits=gate_toks + [t_cl, tail], sig=True)
                    t_a2 = P.op("vector", lambda e, L=L: e.tensor_tensor(out=ss[:, :, 0:L], in0=aa[:, :, 0:L], in1=aa[:, :, 0:L],
                                                                         op=ALU.mult), waits=[ta, tail], sig=True)
                    t_sq = P.op("scalar", lambda e, L=L: e.activation(out=ss[:, :, 0:L], in_=ss[:, :, 0:L], func=AF.Sqrt,
                                                                      scale=-1.0, bias=1.0), waits=[t_a2], sig=True)
                    t_b1 = P.op("vector", lambda e, b=b, L=L: e.tensor_tensor(out=ii[:, :, 0:L], in0=ii[:, :, 0:L], in1=uc[b][:, :, 0:L],
                                                                             op=ALU.mult), waits=gate_toks + [t_a2], sig=True)
                    t_b2 = P.op("vector", lambda e, L=L: e.tensor_tensor(out=ii[:, :, 0:L], in0=ii[:, :, 0:L], in1=ss[:, :, 0:L],
                                                                        op=ALU.mult), waits=[t_b1, t_sq], sig=True)
                    th = t_b2
                    for j in range(2):
                        th = P.op("vector", lambda e, j=j, L=L: e.tensor_tensor_scan(
                            out=hh[:, j, 0:L], data0=aa[:, j, 0:L], data1=ii[:, j, 0:L], initial=carry[:, j:j + 1],
                            op0=ALU.mult, op1=ALU.add), waits=[th, t_h0], sig=True)
                    t_cy = P.op("vector", lambda e, L=L: e.tensor_copy(out=carry[:].unsqueeze(2), in_=hh[:, :, L - 1:L]),
                                waits=[th], sig=True)
                    tail = t_cy
                    if emit:
                        t_y = P.op("vector", lambda e, b=b, L=L: e.tensor_tensor(out=yb[b][:, :, 0:L], in0=hh[:, :, 0:L],
                                                                                in1=gb[b][:, :, 0:L], op=ALU.mult),
                                   waits=[t_cy, t_g, y_free[b]], sig=True)
                        tail = t_y
                        g_free[b] = t_y
                        t0_ = c0 // 128
                        nt = (L + 127) // 128
                        toks = []
                        for j in range(2):
                            kc = 2 * n + j
                            if L % 128 == 0:
                                dstv = XTout[t0_:t0_ + nt, :, kc * 128:(kc + 1) * 128].rearrange("t p c -> p t c")
                                srcv = yb[b][:, j, 0:L].rearrange("p (t c) -> p t c", c=128)
                            else:
                                dstv = XTout[t0_][:, kc * 128:kc * 128 + L]
                                srcv = yb[b][:, j, 0:L]
                            toks.append(P.dma("gpsimd", dstv, srcv, s_y[b][j], waits=[t_y], store=True))
                        y_free[b] = toks
                    u_free[b] = tail
                    if sg == nseg - 1:
                        k = sti % 2
                        sti += 1
                        t_s1 = P.op("gpsimd", lambda e, k=k, b=b, L=L: e.tensor_copy(out=stt[k][:, :, 0:3], in_=ub[b][:, :, L:L + 3]),
                                    waits=[t_u, t_u1, st_free[k]], sig=True)
                        t_s2 = P.op("gpsimd", lambda e, k=k, L=L: e.tensor_copy(out=stt[k][:, :, 3:4], in_=hh[:, :, L - 1:L]),
                                    waits=[th, t_s1], sig=True)
                        tail = [tail, t_s2]
                        u_free[b] = tail
                        st_free[k] = P.dma("gpsimd", STd[sq["st"]][:, 2 * n:2 * n + 2, 0:4], stt[k][:], s_s[k], waits=[t_s2], store=True)
            w_free[wbuf] = last_mm
        P.flush()


def carry_phase(nc, name, STd, flags_in, HALOd, H0d):
    with Phase(nc, name) as P:
        st = P.sb("st", [128, KC, 4], F32)
        fl = P.sb("fl", [128, 16], F32)
        s1, s2, s3, s4 = P.dsem(), P.dsem(), P.dsem(), P.dsem()
        t_f = P.dma("sync", fl[:], flags_in[:, :], s1)
        t_a = P.dma("sync", st[:], STd[0][:, :, 0:4], s2)
        stf = st[:].rearrange("p k c -> p (k c)")
        t_x = P.op("vector", lambda e: e.tensor_scalar(out=stf, in0=stf, scalar1=fl[:, 0:1], scalar2=None, op0=ALU.mult),
                   waits=[t_f, t_a], sig=True)
        P.dma("gpsimd", HALOd[:, :, :], st[:, :, 0:3], s3, waits=[t_x], store=True)
        P.dma("gpsimd", H0d[:, :, :], st[:, :, 3:4], s4, waits=[t_x], store=True)
        P.flush()


def exchange_phase(nc, name, STd, CCin, CCout, flags_in, UT, H0d):
    with Phase(nc, name) as P:
        st = P.sb("st", [128, KC, 4], F32)
        g8 = P.sb("g8", [128, 2, KC * 4], F32)
        fl = P.sb("fl", [128, 16], F32)
        acc = P.sb("acc", [128, KC, 4], F32)
        s1, s2, s4, s5 = P.dsem(), P.dsem(), P.dsem(), P.dsem()
        s3 = P.G.cc
        s6, s7 = P.dsem(), P.dsem()
        t_f = P.dma("sync", fl[:], flags_in[:, :], s5)
        t_a = P.dma("sync", st[:], STd[0][:, :, 0:4], s1)
        t_b = P.dma("sync", CCin[:, :], st[:].rearrange("p k c -> p (k c)"), s2, waits=[t_a])
        tok = s3.next()
        P.q["gpsimd"].append(Op(_flat([t_b]), lambda e: e.collective_compute(
            "AllGather", ALU.bypass, replica_groups=[[0, 1], [2, 3], [4, 5], [6, 7]], ins=[CCin[:, :]], outs=[CCout[:, :]]), s3))
        t_g = P.dma("sync", g8[:], CCout.rearrange("(r p) c -> p r c", p=128), s4, waits=[tok])
        accf = acc[:].rearrange("p k c -> p (k c)")
        t_x = P.op("vector", lambda e: e.tensor_scalar(out=accf, in0=g8[:, 0, :], scalar1=fl[:, 2:3], scalar2=None, op0=ALU.mult),
                   waits=[t_g, t_f], sig=True)
        for r in range(1, 2):
            t_x = P.op("vector", lambda e, r=r: e.scalar_tensor_tensor(out=accf, in0=g8[:, r, :], scalar=fl[:, 2 + r:3 + r], in1=accf,
                                                                       op0=ALU.mult, op1=ALU.add), waits=[t_x], sig=True)
        P.dma("gpsimd", UT[:, :, 509:512], acc[:, :, 0:3], s6, waits=[t_x], store=True)
        P.dma("gpsimd", H0d[:, :, :], acc[:, :, 3:4], s7, waits=[t_x], store=True)
        P.flush()


def router_setup_factory(wrT_in):
    def setup(P):
        st = dict(wr=P.sb("wr", [128, NEXP, D], F32), junk=P.sb("junk", [128, D], F32),
                  lg=[P.sb(f"lg{i}", [128, 8], F32) for i in range(2)], m8=[P.sb(f"m8{i}", [128, 8], F32) for i in range(2)],
                  m1=[P.sb(f"m1{i}", [128, 8], F32) for i in range(2)], m2=[P.sb(f"m2{i}", [128, 8], F32) for i in range(2)],
                  dl=[P.sb(f"dl{i}", [128, 4], F32) for i in range(2)], gs=[P.sb(f"gs{i}", [128, 8], F32) for i in range(2)],
                  sw=P.dsem("wr"), so=[P.dsem(f"rg{i}") for i in range(2)], free=[None, None], tw=None, last=None)
        for e in range(NEXP):
            st["tw"] = P.dma("sync", st["wr"][:, e, :], wrT_in[e:e + 1, :].to_broadcast([128, D]), st["sw"])
        return st
    return setup


def make_router_hook(GATES, tiles):
    def hook(P, st, t, y, t_y):
        b = t % 2
        lg, m8, m1, m2, dl, gs = st["lg"][b], st["m8"][b], st["m1"][b], st["m2"][b], st["dl"][b], st["gs"][b]
        tk = st["last"]
        for e in range(NEXP):
            tk = P.op("vector", lambda en, e=e: en.scalar_tensor_tensor(
                out=st["junk"][:], in0=y[:], scalar=1.0, in1=st["wr"][:, e, :], op0=ALU.mult, op1=ALU.mult,
                accum_out=lg[:, e:e + 1]), waits=[t_y, st["tw"], tk, st["free"][b]], sig=True)
        t8 = P.op("vector", lambda en: en.max(out=m8[:], in_=lg[:]), waits=[tk], sig=True)
        t1 = P.op("vector", lambda en: en.tensor_scalar(out=m1[:], in0=lg[:], scalar1=m8[:, 0:1], scalar2=None, op0=ALU.is_equal),
                  waits=[t8], sig=True)
        t2 = P.op("vector", lambda en: en.tensor_scalar(out=m2[:], in0=lg[:], scalar1=m8[:, 1:2], scalar2=None, op0=ALU.is_equal),
                  waits=[t1], sig=True)
        t3 = P.op("vector", lambda en: en.tensor_tensor(out=dl[:, 0:1], in0=m8[:, 0:1], in1=m8[:, 1:2], op=ALU.subtract),
                  waits=[t2], sig=True)
        t4 = P.op("scalar", lambda en: en.activation(out=dl[:, 1:2], in_=dl[:, 0:1], func=AF.Sigmoid), waits=[t3], sig=True)
        t5 = P.op("scalar", lambda en: en.activation(out=dl[:, 2:3], in_=dl[:, 0:1], func=AF.Sigmoid, scale=-1.0), waits=[t4], sig=True)
        t6 = P.op("vector", lambda en: en.tensor_scalar(out=gs[:], in0=m1[:], scalar1=dl[:, 1:2], scalar2=None, op0=ALU.mult),
                  waits=[t5], sig=True)
        t7 = P.op("vector", lambda en: en.scalar_tensor_tensor(out=gs[:], in0=m2[:], scalar=dl[:, 2:3], in1=gs[:], op0=ALU.mult,
                                                              op1=ALU.add), waits=[t6], sig=True)
        st["last"] = t7
        tile = tiles[t]
        st["free"][b] = P.dma("gpsimd", GATES[tile * 128:(tile + 1) * 128, :], gs[:], st["so"][b], waits=[t7], store=True)
        return [t7, st["free"][b]]
    return hook


def build_program(S_OWN, upto=99):
    assert S_OWN % 512 == 0
    NX = 512 + 2 * S_OWN + 256
    NXT = NX // 128
    NPAIR = S_OWN // 128
    RT = list(range(4, NXT))
    RT2 = list(range(4 + NPAIR, NXT))
    TS = [4 + 2 * NPAIR, 4 + 2 * NPAIR + 1]
    OWN0 = 512 + S_OWN

    nc = bass.Bass("TRN2", target_bir_lowering=False)

    def din(name, shape, dt=F32):
        return nc.dram_tensor(name, list(shape), dt, kind="ExternalInput").ap()

    def dout(name, shape, dt=F32):
        return nc.dram_tensor(name, list(shape), dt, kind="ExternalOutput").ap()

    def dscr(name, shape, dt):
        return nc.dram_tensor(name, list(shape), dt, kind="ExternalOutput" if DEBUG else "Internal").ap()

    xin = din("xin", [NX, D])
    cache_k = din("cache_k", [2, 512, D])
    cache_v = din("cache_v", [2, 512, D])
    ident_in = din("ident", [128, 128])
    bt_in = din("bt", [5, 128, NH * 128])
    flags = din("flags", [128, 16])
    ln_g = din("ln_g", [4, D])
    ln_b = din("ln_b", [4, D])
    w_attn_in = din("w_attn_in", [D, 3 * D])
    w_attn_out = din("w_attn_out", [D, D])
    w_ffn_in = din("w_ffn_in", [D, 2 * DFF])
    w_ffn_out = din("w_ffn_out", [DFF, D])
    w_rnn_in = din("w_rnn_in", [D, 2 * D])
    w_rnn_out = din("w_rnn_out", [D, D])
    w_rg_a = din("w_rg_a", [8, 256, 256])
    w_rg_x = din("w_rg_x", [8, 256, 256])
    pf_in = din("pf", [128, 8, KC])
    sconv_in = din("sconv", [256, D])
    zeros_in = din("zeros", [128, D])
    wrT_in = din("wrT", [NEXP, D])
    if upto >= 8:
        w_moe_in = din("w_moe_in", [NEXP, D, 2 * DFF])
        w_moe_out = din("w_moe_out", [NEXP, DFF, D])

    o_newk = dout("o_newk", [512 + 32, D])
    o_newv = dout("o_newv", [512 + 32, D])
    o_y = dout("o_y", [S_OWN + 256, D])
    o_state = dout("o_state", [3, 4, D])

    XT = dscr("XT", [NXT, 128, KC * 128], BF16)
    QKV = dscr("QKV", [NX, 3 * D], F32)
    QT = dscr("QT", [NXT, 128, KC * 128], BF16)
    KT = dscr("KT", [NXT + 8, 128, KC * 128], BF16)
    V1 = dscr("V1", [NXT + 8, 128, NH * VS], BF16)
    OA = dscr("OA", [NX, D], F32)
    M = dscr("M", [NX, D], F32)
    X1 = dscr("X1", [NX, D], F32)
    X2 = dscr("X2", [NX, D], F32)
    H = dscr("H", [NX, DFF], F32)
    HT = dscr("HT", [NXT, 128, 44 * 128], BF16)
    X3 = dscr("X3", [NX, D], F32)
    UG = dscr("UG", [NX, 2 * D], F32)
    UT = dscr("UT", [128, KC, NX], F32)
    GGT = dscr("GGT", [128, KC, NX], F32)
    SCT = dscr("SCT", [128, KC, 256], F32)
    STd = dscr("STd", [3, 128, KC, 128], F32)
    STo = dscr("STo", [3, 128, D], F32)
    H0d = dscr("H0d", [128, KC, 1], F32)
    HALOd = dscr("HALOd", [128, KC, 3], F32)
    CCin = nc.dram_tensor("CCin", [128, KC * 4], F32, kind="Internal").ap()
    CCout = nc.dram_tensor("CCout", [2 * 128, KC * 4], F32, kind="Internal").ap()
    GATES = dscr("GATES", [NX, 8], F32)
    FA = dscr("FA", [NX, D], F32)

    def rows(ap, t, c0=0, c1=None):
        return ap[t * 128:(t + 1) * 128, c0:(c1 if c1 is not None else ap.shape[1])]

    transpose_phase(nc, "t0", ident_in, [(rows(xin, t), XT[t]) for t in range(NXT)], KC)
    if upto >= 2:
        gemm_phase(nc, "g0", XT, list(range(NXT)), KC, w_attn_in, [[c] for c in range(0, 3 * D, 512)],
                   StoreEpi(QKV, lambda blk, s: blk * 512))
    if upto >= 3:
        transpose_phase(nc, "tq", ident_in, [(rows(QKV, t, 0, D), QT[t]) for t in RT], KC, scale=0.125)
        jobs = [(rows(QKV, t, D, 2 * D), KT[t]) for t in range(NXT)]
        jobs += [(cache_k[s, j * 128:(j + 1) * 128, :], KT[NXT + 4 * s + j]) for s in range(2) for j in range(4)]
        transpose_phase(nc, "tk", ident_in, jobs, KC)
        jobs = [(rows(QKV, t, 2 * D, 3 * D), V1[t], (3 if t < 4 else (0 if t < 4 + NPAIR else (1 if t in TS else None))))
                for t in range(NXT)]
        jobs += [(cache_v[s, j * 128:(j + 1) * 128, :], V1[NXT + 4 * s + j], None) for s in range(2) for j in range(4)]
        v1_phase(nc, "v1", jobs, flags)
        r0 = OWN0 + S_OWN - 512
        cp = [(o_newk[0:512, :], QKV[r0:r0 + 512, D:2 * D]), (o_newv[0:512, :], QKV[r0:r0 + 512, 2 * D:3 * D])]
        for s in range(2):
            rs = TS[s] * 128
            cp.append((o_newk[512 + 16 * s:528 + 16 * s, :], QKV[rs:rs + 16, D:2 * D]))
            cp.append((o_newv[512 + 16 * s:528 + 16 * s, :], QKV[rs:rs + 16, 2 * D:3 * D]))
        copy_phase(nc, "cpkv", cp)
    if upto >= 4:
        units = [(4 + u, [u + j for j in range(5)]) for u in range(2 * NPAIR)]
        units += [(TS[s], [NXT + 4 * s + j for j in range(4)] + [TS[s]]) for s in range(2)]
        attn_phase(nc, "att", units, ident_in, bt_in, QT, KT, V1, OA)
    if upto >= 5:
        transpose_phase(nc, "toa", ident_in, [(rows(OA, t), XT[t]) for t in RT], KC)
        gemm_phase(nc, "go", XT, RT, KC, w_attn_out, [[c] for c in range(0, D, 512)], StoreEpi(M, lambda blk, s: blk * 512))
        ln_phase(nc, "ln0", [(rows(xin, t), rows(M, t), [(rows(X1, t), 128)]) for t in RT], ln_g[0:1, :], ln_b[0:1, :])
    if upto >= 6:
        transpose_phase(nc, "tx1", ident_in, [(rows(X1, t), XT[t]) for t in RT], KC)
        gemm_phase(nc, "gf1", XT, RT, KC, w_ffn_in, [[c, DFF + c] for c in range(0, DFF, 512)], SwiGLUEpi(H))
        transpose_phase(nc, "th", ident_in, [(rows(H, t), HT[t]) for t in RT], 44)
        gemm_phase(nc, "gf2", HT, RT, 44, w_ffn_out, [[c] for c in range(0, D, 512)], StoreEpi(M, lambda blk, s: blk * 512))
        ln_phase(nc, "ln1", [(rows(X1, t), rows(M, t), [(rows(X2, t), 128)]) for t in RT], ln_g[1:2, :], ln_b[1:2, :])
    if upto >= 7:
        transpose_phase(nc, "tx2", ident_in, [(rows(X2, t), XT[t]) for t in RT], KC)
        gemm_phase(nc, "gr1", XT, RT, KC, w_rnn_in, [[c] for c in range(0, 2 * D, 512)], StoreEpi(UG, lambda blk, s: blk * 512))
        jobs = [(rows(UG, t, 0, D), UT[:, :, t * 128:(t + 1) * 128]) for t in RT]
        transpose_phase(nc, "tu", ident_in, jobs, KC, out_dt=F32, dst3=True)
        jobs = [(None, GGT[:, :, t * 128:(t + 1) * 128]) for t in RT2]
        transpose_phase(nc, "tg", ident_in, jobs, KC, out_dt=F32, dst3=True,
                        prep=make_gelu_prep([rows(UG, t, D, 2 * D) for t in RT2]), prep_setup=gelu_prep_setup)
        jobs = [(sconv_in[s * 128:(s + 1) * 128, :], SCT[:, :, s * 128:(s + 1) * 128]) for s in range(2)]
        transpose_phase(nc, "tsc", ident_in, jobs, KC, out_dt=F32, dst3=True)
        ZER = zeros_in.rearrange("p (k c) -> p k c", c=128)
        copy_phase(nc, "zst", [(STd[i].rearrange("p k c -> p (k c)"), zeros_in[:, :]) for i in range(3)])
        seq_p1 = [dict(col0=512, L=S_OWN, halo=ZER[:, :, 0:3], h0=ZER[:, :, 3:4], st=0)]
        scan_phase(nc, "sc1", seq_p1, UT, GGT, XT, STd, pf_in, w_rg_a, w_rg_x, emit=False)
        carry_phase(nc, "cry", STd, flags, HALOd, H0d)
        seq_p2 = [dict(col0=OWN0, L=S_OWN, halo=HALOd, h0=H0d, st=0)]
        seq_p2 += [dict(col0=TS[s] * 128, L=16, halo=SCT[:, :, s * 128:s * 128 + 3], h0=SCT[:, :, s * 128 + 3:s * 128 + 4], st=1 + s)
                   for s in range(2)]
        scan_phase(nc, "sc2", seq_p2, UT, GGT, XT, STd, pf_in, w_rg_a, w_rg_x, emit=True)
        transpose_phase(nc, "tst", ident_in, [(STd[i].rearrange("p k c -> p (k c)"), STo[i]) for i in range(3)], KC, out_dt=F32)
        copy_phase(nc, "cst", [(o_state[i], STo[i, 0:4, :]) for i in range(3)])
        gemm_phase(nc, "gr2", XT, RT2, KC, w_rnn_out, [[c] for c in range(0, D, 512)], StoreEpi(M, lambda blk, s: blk * 512))
        ln_phase(nc, "ln2", [(rows(X2, t), rows(M, t), [(rows(X3, t), 128)]) for t in RT2], ln_g[2:3, :], ln_b[2:3, :],
                 hook=make_router_hook(GATES, RT2), hook_setup=router_setup_factory(wrT_in))
    if upto >= 8:
        transpose_phase(nc, "tx3", ident_in, [(rows(X3, t), XT[t]) for t in RT2], KC)
        for e in range(NEXP):
            gemm_phase(nc, f"gm1_{e}", XT, RT2, KC, w_moe_in[e], [[c, DFF + c] for c in range(0, DFF, 512)], SwiGLUEpi(H))
            transpose_phase(nc, f"thm{e}", ident_in, [(rows(H, t), HT[t]) for t in RT2], 44)
            gemm_phase(nc, f"gm2_{e}", HT, RT2, 44, w_moe_out[e], [[c] for c in range(0, D, 512)], AccEpi(FA, GATES, e))
        jobs = []
        for t in RT2:
            r0 = (t - 4 - NPAIR) * 128
            jobs.append((rows(X3, t), rows(FA, t), [(o_y[r0:r0 + 128, :], 128)]))
        ln_phase(nc, "ln3", jobs, ln_g[3:4, :], ln_b[3:4, :])
    elif upto >= 7:
        copy_phase(nc, "cpy", [(o_y[:, :], X3[512:NX, :])])
    elif upto >= 6:
        copy_phase(nc, "cpy", [(o_y[:, :], X2[512:NX, :])])
    return nc, dict(NX=NX, NXT=NXT, NPAIR=NPAIR, TS=TS, OWN0=OWN0)


def _bias_table(rel_bias):
    kw = np.arange(640)[:, None]
    qi = np.arange(128)[None, :]
    d = np.clip(qi + 512 - kw, -256, 256) + 256
    tab = rel_bias[:, d]
    tab = np.transpose(tab, (1, 0, 2)).copy()
    dc = (qi // 64 + 8) - kw // 64
    mask = (dc >= 0) & (dc <= 8)
    tab = np.where(mask[:, None, :], tab, np.float32(NEG)).astype(np.float32)
    return np.ascontiguousarray(tab.reshape(5, 128, NH * 128))


def make_in_maps(inp, S_OWN, upto=99):
    xp = inp["x_prompt"]
    xs = inp["x_sample"]
    B = xp.shape[0]
    n_cores = 2 * B
    maps = []
    bt = _bias_table(inp["rel_bias"][0])
    ident = np.eye(128, dtype=np.float32)
    vecs = [inp["conv_w"][0, 0], inp["conv_w"][0, 1], inp["conv_w"][0, 2], inp["conv_w"][0, 3], inp["conv_b"][0],
            inp["b_rg_a"][0].reshape(-1), inp["b_rg_x"][0].reshape(-1), inp["rg_lambda"][0]]
    pf = np.ascontiguousarray(np.stack([v.reshape(KC, 128).T for v in vecs], axis=1)).astype(np.float32)
    zeros = np.zeros((128, D), np.float32)
    wrT = np.ascontiguousarray(inp["w_router"][0].T)
    for c in range(n_cores):
        b, half = c // 2, c % 2
        NX = 512 + 2 * S_OWN + 256
        xin = np.zeros((NX, D), np.float32)
        if half == 1:
            xin[512:512 + S_OWN] = xp[b, 0:S_OWN]
        o0 = 512 + S_OWN
        xin[o0:o0 + S_OWN] = xp[b, half * S_OWN:(half + 1) * S_OWN]
        xin[o0 + S_OWN:o0 + S_OWN + 16] = xs[2 * c]
        xin[o0 + S_OWN + 128:o0 + S_OWN + 144] = xs[2 * c + 1]
        fl = np.zeros((128, 16), np.float32)
        fl[:, 0] = float(half)
        fl[:16, 1] = 1.0
        if half == 1:
            fl[:, 2] = 1.0
        sconv = np.zeros((256, D), np.float32)
        for s_ in range(2):
            sconv[s_ * 128:s_ * 128 + 3] = inp["state_conv"][0, 2 * c + s_]
            sconv[s_ * 128 + 3] = inp["state_h"][0, 2 * c + s_]
        m = {
            "xin": xin,
            "cache_k": np.ascontiguousarray(inp["cache_k"][0, 2 * c:2 * c + 2].reshape(2, 512, D)),
            "cache_v": np.ascontiguousarray(inp["cache_v"][0, 2 * c:2 * c + 2].reshape(2, 512, D)),
            "ident": ident,
            "bt": bt,
            "flags": fl,
            "ln_g": np.ascontiguousarray(inp["ln_g"].reshape(4, D)),
            "ln_b": np.ascontiguousarray(inp["ln_b"].reshape(4, D)),
            "w_attn_in": inp["w_attn_in"][0],
            "w_attn_out": inp["w_attn_out"][0],
            "w_ffn_in": inp["w_ffn_in"][0],
            "w_ffn_out": inp["w_ffn_out"][0],
            "w_rnn_in": inp["w_rnn_in"][0],
            "w_rnn_out": inp["w_rnn_out"][0],
            "w_rg_a": inp["w_rg_a"][0],
            "w_rg_x": inp["w_rg_x"][0],
            "pf": pf,
            "sconv": sconv,
            "zeros": zeros,
            "wrT": wrT,
        }
        if upto >= 8:
            m["w_moe_in"] = inp["w_moe_in"][0]
            m["w_moe_out"] = inp["w_moe_out"][0]
        maps.append(m)
    return maps


def kernel(**inputs):
    inputs = {k: np.asarray(v) for k, v in inputs.items()}
    B, SEQ, _ = inputs["x_prompt"].shape
    S_OWN = SEQ // 2
    upto = int(os.environ.get("MK_UPTO", "8"))
    nc, meta = build_program(S_OWN, upto=upto)
    maps = make_in_maps(inputs, S_OWN, upto)
    res = run_bass_kernel_spmd(nc, maps, core_ids=list(range(8)))
    r = res.results
    NS = inputs["x_sample"].shape[0]
    TSEQ = inputs["x_sample"].shape[1]
    y_prompt = np.zeros((B, SEQ, D), np.float32)
    y_sample = np.zeros((NS, TSEQ, D), np.float32)
    nk_p = np.zeros((1, B, 512, NH, HD), np.float32)
    nv_p = np.zeros((1, B, 512, NH, HD), np.float32)
    nk_s = np.zeros((1, NS, TSEQ, NH, HD), np.float32)
    nv_s = np.zeros((1, NS, TSEQ, NH, HD), np.float32)
    nc_p = np.zeros((1, B, 3, D), np.float32)
    nh_p = np.zeros((1, B, D), np.float32)
    nc_s = np.zeros((1, NS, 3, D), np.float32)
    nh_s = np.zeros((1, NS, D), np.float32)
    for c in range(8):
        b, half = c // 2, c % 2
        y = np.asarray(r[c]["o_y"])
        y_prompt[b, half * S_OWN:(half + 1) * S_OWN] = y[0:S_OWN]
        y_sample[2 * c] = y[S_OWN:S_OWN + 16]
        y_sample[2 * c + 1] = y[S_OWN + 128:S_OWN + 144]
        k = np.asarray(r[c]["o_newk"])
        v = np.asarray(r[c]["o_newv"])
        if half == 1:
            nk_p[0, b] = k[0:512].reshape(512, NH, HD)
            nv_p[0, b] = v[0:512].reshape(512, NH, HD)
        for s in range(2):
            nk_s[0, 2 * c + s] = k[512 + 16 * s:528 + 16 * s].reshape(16, NH, HD)
            nv_s[0, 2 * c + s] = v[512 + 16 * s:528 + 16 * s].reshape(16, NH, HD)
        st = np.asarray(r[c]["o_state"])
        if half == 1:
            nc_p[0, b] = st[0, 0:3]
            nh_p[0, b] = st[0, 3]
        for s in range(2):
            nc_s[0, 2 * c + s] = st[1 + s, 0:3]
            nh_s[0, 2 * c + s] = st[1 + s, 3]
    return (y_prompt, y_sample, nk_p, nv_p, nk_s, nv_s, nc_p, nh_p, nc_s, nh_s)
```

```python
import os
from contextlib import ExitStack
import numpy as np
import concourse.bass as bass
import concourse.mybir as mybir
from concourse.bass_utils import run_bass_kernel_spmd

F32 = mybir.dt.float32
BF16 = mybir.dt.bfloat16
AF = mybir.ActivationFunctionType
ALU = mybir.AluOpType

D = 2048
KC = 16
NH = 32
HD = 64
DFF = 5632
NEXP = 8
ALPHA = (2.0 * 2) ** 0.25
LN_EPS = 1e-5
NEG = -30000.0
VS = 66
ENGS = ("sync", "scalar", "vector", "gpsimd", "tensor")
DEBUG = bool(int(os.environ.get("MK_DEBUG", "0")))
ATT_STAGE = int(os.environ.get("MK_ATT_STAGE", "3"))
NOXCH = bool(int(os.environ.get("MK_NOXCH", "0")))


class Sem:
    def __init__(self, h, step):
        self.h, self.step, self.n = h, step, 0

    def next(self):
        self.n += self.step
        return (self.h, self.n)


def _flat(ws):
    out = []
    for w in ws:
        if w is None:
            continue
        if isinstance(w, list):
            out.extend(_flat(w))
        else:
            out.append(w)
    return out


class Op:
    __slots__ = ("waits", "build", "sig")

    def __init__(self, waits, build, sig):
        self.waits, self.build, self.sig = waits, build, sig


class GlobalSems:
    def __init__(self, nc):
        self.es = ExitStack()
        self.es.__enter__()
        mk = lambda nm: self.es.enter_context(nc.semaphore(nm))
        self.esem = {e: Sem(mk("e_" + e), 1) for e in ("scalar", "vector", "gpsimd", "tensor")}
        self.cc = Sem(mk("cc"), 16)
        self.pool = {"sync": [Sem(mk(f"ds{i}"), 16) for i in range(20)],
                     "gpsimd": [Sem(mk(f"dp{i}"), 16) for i in range(10)]}


class LazySem:
    def __init__(self):
        self.s = None


_G = {}


class Phase:
    def __init__(self, nc, name):
        self.nc, self.name = nc, name
        self.es = ExitStack()
        self.q = {e: [] for e in ENGS}
        self.store_toks = []
        if id(nc) not in _G:
            _G.clear()
            _G[id(nc)] = GlobalSems(nc)
        self.G = _G[id(nc)]
        self.esem = self.G.esem
        self.used = {"sync": 0, "gpsimd": 0}

    def __enter__(self):
        self.es.__enter__()
        return self

    def __exit__(self, *a):
        return self.es.__exit__(*a)

    def dsem(self, name=None):
        return LazySem()

    def bind(self, ls, eng):
        if ls.s is None:
            q = "gpsimd" if eng == "gpsimd" else "sync"
            ls.s = self.G.pool[q][self.used[q]]
            self.used[q] += 1
        return ls.s

    def sb(self, name, shape, dt):
        return self.es.enter_context(self.nc.sbuf_tensor(f"{self.name}_{name}", list(shape), dt))

    def ps(self, name, shape, dt=F32):
        return self.es.enter_context(self.nc.psum_tensor(f"{self.name}_{name}", list(shape), dt))

    def op(self, eng, build, waits=(), sig=False):
        o = Op(_flat(waits), build, None)
        tok = None
        if sig:
            s = self.esem[eng]
            o.sig = s
            tok = s.next()
        self.q[eng].append(o)
        return tok

    def dma(self, eng, out, in_, dsem, waits=(), store=False):
        dsem = self.bind(dsem, eng)
        def build(e):
            try:
                return e.dma_start(out=out, in_=in_)
            except ValueError:
                return e.dma_start(out=out, in_=in_, allow_slow_non_contiguous=True)

        o = Op(_flat(waits), build, dsem)
        tok = dsem.next()
        self.q[eng].append(o)
        if store:
            self.store_toks.append(tok)
        return tok

    def sig_last(self, eng):
        q = self.q[eng]
        s = self.esem[eng]
        for o in reversed(q):
            if o.build is not None:
                if o.sig is None:
                    o.sig = s
                    return s.next()
                if o.sig is s:
                    return (s.h, s.n)
                break
        return None

    def flush(self):
        fin = []
        for e in ("scalar", "vector", "gpsimd", "tensor"):
            t = self.sig_last(e)
            if t is not None:
                fin.append(t)
        st = {}
        for (h, v) in self.store_toks:
            k = id(h)
            if k not in st or st[k][1] < v:
                st[k] = (h, v)
        fin = fin + list(st.values())
        for e in ENGS:
            self.q[e].append(Op(list(fin), None, None))
        nc = self.nc
        with nc.Block() as blk:
            for e in ENGS:
                ops = self.q[e]

                def run(eng, ops=ops):
                    seen = {}
                    for o in ops:
                        for (h, v) in o.waits:
                            k = id(h)
                            if seen.get(k, -1) >= v:
                                continue
                            seen[k] = v
                            eng.wait_ge(h, v)
                        if o.build is None:
                            continue
                        ins = o.build(eng)
                        if o.sig is not None:
                            ins.then_inc(o.sig.h, o.sig.step)

                getattr(blk, e)(run)


def _evac(P, i, out, in_, waits, scale=None):
    if i % 2 == 0:
        if scale is None:
            return P.op("vector", lambda e: e.tensor_copy(out=out, in_=in_), waits=waits, sig=True)
        return P.op("vector", lambda e: e.tensor_scalar(out=out, in0=in_, scalar1=float(scale), scalar2=None,
                                                        op0=ALU.mult), waits=waits, sig=True)
    return P.op("scalar", lambda e: e.activation(out=out, in_=in_, func=AF.Copy,
                                                 scale=(1.0 if scale is None else float(scale))),
                waits=waits, sig=True)


def transpose_phase(nc, name, ident_in, jobs, KCk, out_dt=BF16, scale=None, prep=None, prep_setup=None, dst3=False):
    with Phase(nc, name) as P:
        ident = P.sb("ident", [128, 128], F32)
        xt = [P.sb(f"x{i}", [128, KCk * 128], F32) for i in range(2)]
        xT = [P.sb(f"xT{i}", [128, KCk * 128], out_dt) for i in range(2)]
        pst = [P.ps(f"ps{i}", [128, 512], F32) for i in range(4)]
        s_id = P.dsem("id")
        s_x = [P.dsem(f"x{i}") for i in range(2)]
        s_o = [P.dsem(f"o{i}") for i in range(2)]
        t_id = P.dma("sync", ident[:], ident_in[:, :], s_id)
        st = prep_setup(P) if prep_setup is not None else None
        x_free = [None, None]
        xT_free = [None, None]
        ps_free = [None] * 4
        NG = KCk // 4
        gi = 0
        for t, (src, dst) in enumerate(jobs):
            b = t % 2
            if prep is None:
                t_ld = P.dma("sync", xt[b][:], src, s_x[b], waits=[x_free[b]])
            else:
                t_ld = prep(P, st, t, xt[b], x_free[b])
            ev = []
            tk = None
            for g in range(NG):
                pb = gi % 4
                gi += 1
                for j in range(4):
                    kc = g * 4 + j
                    tk = P.op("tensor", lambda e, pb=pb, j=j, kc=kc, b=b: e.transpose(
                        pst[pb][:, j * 128:(j + 1) * 128], xt[b][:, kc * 128:(kc + 1) * 128], ident[:]),
                        waits=[t_ld, t_id, ps_free[pb]] if j == 0 else [], sig=(j == 3))
                te = _evac(P, g, xT[b][:, g * 512:(g + 1) * 512], pst[pb][:], [tk, xT_free[b]], scale)
                ps_free[pb] = te
                ev.append(te)
            x_free[b] = tk
            srcv = xT[b][:].rearrange("p (k t) -> p k t", t=128) if dst3 else xT[b][:]
            xT_free[b] = P.dma("gpsimd", dst, srcv, s_o[b], waits=ev, store=True)
        P.flush()


def gelu_prep_setup(P):
    return dict(g=[P.sb(f"gg{i}", [128, D], F32) for i in range(2)], t=[P.sb(f"gt{i}", [128, D], F32) for i in range(2)],
                s=[P.dsem(f"gs{i}") for i in range(2)], free=[None, None])


def make_gelu_prep(srcs):
    def prep(P, st, t, dst_tile, dst_free):
        b = t % 2
        g, tt = st["g"][b], st["t"][b]
        t_l = P.dma("sync", g[:], srcs[t], st["s"][b], waits=[st["free"][b]])
        t_1 = P.op("scalar", lambda e: e.activation(out=tt[:], in_=g[:], func=AF.Square), waits=[t_l, st["free"][b]], sig=True)
        t_2 = P.op("vector", lambda e: e.tensor_scalar(out=tt[:], in0=tt[:], scalar1=0.044715, scalar2=1.0,
                                                       op0=ALU.mult, op1=ALU.add), waits=[t_1], sig=True)
        t_3 = P.op("vector", lambda e: e.tensor_tensor(out=tt[:], in0=tt[:], in1=g[:], op=ALU.mult), waits=[t_2], sig=True)
        t_4 = P.op("scalar", lambda e: e.activation(out=tt[:], in_=tt[:], func=AF.Sigmoid, scale=1.5957691216057308),
                   waits=[t_3], sig=True)
        t_5 = P.op("vector", lambda e: e.tensor_tensor(out=dst_tile[:], in0=tt[:], in1=g[:], op=ALU.mult),
                   waits=[t_4, dst_free], sig=True)
        st["free"][b] = t_5
        return t_5
    return prep


class AccEpi:
    def __init__(self, FA, GATES, e):
        self.FA, self.G, self.e = FA, GATES, e

    def setup(self, P):
        self.acc = [P.sb(f"acc{i}", [128, 512], F32) for i in range(3)]
        self.gt = [P.sb(f"gt{i}", [128, 8], F32) for i in range(3)]
        self.sa = [P.dsem(f"sa{i}") for i in range(3)]
        self.sg = [P.dsem(f"sg{i}") for i in range(3)]
        self.so = [P.dsem(f"so{i}") for i in range(3)]
        self.free = [None] * 3
        self.i = 0

    def __call__(self, P, tile, blk, pss, ready):
        k = self.i % 3
        self.i += 1
        e = self.e
        dst = self.FA[tile * 128:(tile + 1) * 128, blk * 512:(blk + 1) * 512]
        t_g = P.dma("sync", self.gt[k][:], self.G[tile * 128:(tile + 1) * 128, :], self.sg[k], waits=[self.free[k]])
        if e == 0:
            t_o = P.op("vector", lambda en: en.tensor_scalar(out=self.acc[k][:], in0=pss[0][:], scalar1=self.gt[k][:, e:e + 1],
                                                             scalar2=None, op0=ALU.mult), waits=[ready, t_g, self.free[k]], sig=True)
        else:
            t_a = P.dma("sync", self.acc[k][:], dst, self.sa[k], waits=[self.free[k]])
            t_o = P.op("vector", lambda en: en.scalar_tensor_tensor(out=self.acc[k][:], in0=pss[0][:], scalar=self.gt[k][:, e:e + 1],
                                                                    in1=self.acc[k][:], op0=ALU.mult, op1=ALU.add),
                       waits=[ready, t_g, t_a], sig=True)
        self.free[k] = P.dma("gpsimd", dst, self.acc[k][:], self.so[k], waits=[t_o], store=True)
        return [t_o]


class StoreEpi:
    def __init__(self, Y, col_of_block):
        self.Y, self.col_of_block = Y, col_of_block

    def setup(self, P):
        self.ob = [P.sb(f"ob{i}", [128, 512], F32) for i in range(4)]
        self.so = [P.dsem(f"so{i}") for i in range(4)]
        self.free = [None] * 4
        self.i = 0

    def __call__(self, P, tile, blk, pss, ready):
        done = []
        for s, ps in enumerate(pss):
            k = self.i % 4
            self.i += 1
            te = _evac(P, self.i, self.ob[k][:], ps[:], [ready, self.free[k]])
            c0 = self.col_of_block(blk, s)
            self.free[k] = P.dma("gpsimd", self.Y[tile * 128:(tile + 1) * 128, c0:c0 + 512], self.ob[k][:], self.so[k],
                                 waits=[te], store=True)
            done.append(te)
        return done


def gemm_phase(nc, name, XT, tiles, KCk, W, blocks, epi):
    nsub = len(blocks[0])
    with Phase(nc, name) as P:
        wst = [P.sb(f"wst{i}", [128, 4, 512], F32) for i in range(3)]
        wb = [P.sb(f"wb{i}", [128, nsub * KCk, 512], BF16) for i in range(2)]
        NXB = 4
        xb = [P.sb(f"xb{i}", [128, KCk * 128], BF16) for i in range(NXB)]
        NPS = 8 if nsub == 1 else 4
        ps = [[P.ps(f"ps{i}_{s}", [128, 512], F32) for s in range(nsub)] for i in range(NPS)]
        s_w = [P.dsem(f"w{i}") for i in range(3)]
        s_x = [P.dsem(f"x{i}") for i in range(NXB)]
        epi.setup(P)
        wst_free = [None] * 3
        wb_free = [None] * 2
        xb_free = [None] * NXB
        ps_free = [None] * NPS
        wi = 0
        xi = 0
        pi = 0
        for bi, cols in enumerate(blocks):
            wbuf = bi % 2
            cast_toks = []
            for s, c0 in enumerate(cols):
                for k0 in range(0, KCk, 4):
                    k = wi % 3
                    wi += 1
                    src = W[k0 * 128:(k0 + 4) * 128, c0:c0 + 512].rearrange("(k p) n -> p k n", p=128)
                    t_ld = P.dma("sync", wst[k][:], src, s_w[k], waits=[wst_free[k]])
                    dst = wb[wbuf][:, s * KCk + k0:s * KCk + k0 + 4, :]
                    eng = ("vector", "gpsimd", "scalar")[wi % 3]
                    if eng == "scalar":
                        tc_ = P.op("scalar", lambda e, dst=dst, k=k: e.activation(out=dst, in_=wst[k][:], func=AF.Copy),
                                   waits=[t_ld, wb_free[wbuf]], sig=True)
                    else:
                        tc_ = P.op(eng, lambda e, dst=dst, k=k: e.tensor_copy(out=dst, in_=wst[k][:]),
                                   waits=[t_ld, wb_free[wbuf]], sig=True)
                    wst_free[k] = tc_
                    cast_toks.append(tc_)
            last_mm = None
            for tile in tiles:
                xk = xi % NXB
                xi += 1
                t_x = P.dma("sync", xb[xk][:], XT[tile], s_x[xk], waits=[xb_free[xk]])
                pk = pi % NPS
                pi += 1
                for s in range(nsub):
                    for kc in range(KCk):
                        last_mm = P.op("tensor", lambda e, pk=pk, s=s, kc=kc, xk=xk, wbuf=wbuf: e.matmul(
                            ps[pk][s][:], xb[xk][:, kc * 128:(kc + 1) * 128], wb[wbuf][:, s * KCk + kc, :],
                            start=(kc == 0), stop=(kc == KCk - 1)),
                            waits=([t_x, ps_free[pk]] + cast_toks) if (s == 0 and kc == 0) else [],
                            sig=(s == nsub - 1 and kc == KCk - 1))
                xb_free[xk] = last_mm
                done = epi(P, tile, bi, ps[pk], last_mm)
                ps_free[pk] = done[-1] if len(done) == 1 else None
                if len(done) > 1:
                    ps_free[pk] = done
            wb_free[wbuf] = last_mm
        P.flush()


def ln_phase(nc, name, jobs, g_row, b_row, hook=None, hook_setup=None):
    with Phase(nc, name) as P:
        gt = P.sb("g", [128, D], F32)
        bt = P.sb("b", [128, D], F32)
        xa = [P.sb(f"xa{i}", [128, D], F32) for i in range(2)]
        ma = [P.sb(f"ma{i}", [128, D], F32) for i in range(2)]
        ya = [P.sb(f"ya{i}", [128, D], F32) for i in range(2)]
        st = [P.sb(f"st{i}", [128, 4, 6], F32) for i in range(2)]
        mv = [P.sb(f"mv{i}", [128, 4], F32) for i in range(2)]
        s_c = P.dsem("c")
        s_x = [P.dsem(f"x{i}") for i in range(2)]
        s_m = [P.dsem(f"m{i}") for i in range(2)]
        s_o = [P.dsem(f"o{i}") for i in range(2)]
        P.dma("sync", gt[:], g_row.to_broadcast([128, D]), s_c)
        t_c = P.dma("sync", bt[:], b_row.to_broadcast([128, D]), s_c)
        hs = hook_setup(P) if hook_setup is not None else None
        x_free = [None, None]
        y_free = [None, None]
        for t, (xs, ms, dsts) in enumerate(jobs):
            b = t % 2
            t_x = P.dma("sync", xa[b][:], xs, s_x[b], waits=[x_free[b]])
            t_m = P.dma("sync", ma[b][:], ms, s_m[b], waits=[x_free[b]])
            t_z = P.op("vector", lambda e, b=b: e.scalar_tensor_tensor(out=ma[b][:], in0=xa[b][:], scalar=float(ALPHA),
                                                                       in1=ma[b][:], op0=ALU.mult, op1=ALU.add),
                       waits=[t_x, t_m], sig=True)
            t_s = []
            for j in range(4):
                t_s.append(P.op("vector", lambda e, b=b, j=j: e.bn_stats(out=st[b][:, j, :], in_=ma[b][:, j * 512:(j + 1) * 512]),
                                waits=[t_z] if j == 0 else [], sig=(j == 3)))
            t_a = P.op("vector", lambda e, b=b: e.bn_aggr(out=mv[b][:, 0:2], in_=st[b][:].rearrange("p a b -> p (a b)")),
                       waits=[t_s[-1]], sig=True)
            t_v = P.op("vector", lambda e, b=b: e.tensor_scalar(out=mv[b][:, 2:3], in0=mv[b][:, 1:2], scalar1=float(LN_EPS),
                                                                scalar2=None, op0=ALU.add), waits=[t_a], sig=True)
            t_sq = P.op("scalar", lambda e, b=b: e.activation(out=mv[b][:, 2:3], in_=mv[b][:, 2:3], func=AF.Sqrt),
                        waits=[t_v], sig=True)
            t_r = P.op("vector", lambda e, b=b: e.reciprocal(out=mv[b][:, 3:4], in_=mv[b][:, 2:3]), waits=[t_sq], sig=True)
            t_n0 = P.op("vector", lambda e, b=b: e.tensor_scalar(out=xa[b][:], in0=ma[b][:], scalar1=mv[b][:, 0:1],
                                                             scalar2=mv[b][:, 3:4], op0=ALU.subtract, op1=ALU.mult),
                        waits=[t_r], sig=True)
            t_n = P.op("vector", lambda e, b=b: e.tensor_tensor(out=xa[b][:], in0=xa[b][:], in1=gt[:], op=ALU.mult),
                       waits=[t_c, t_n0], sig=True)
            t_y = P.op("gpsimd", lambda e, b=b: e.tensor_tensor(out=ya[b][:], in0=xa[b][:], in1=bt[:], op=ALU.add),
                       waits=[t_n, y_free[b], t_c], sig=True)
            x_free[b] = t_y
            toks = []
            for (dst, nrows) in dsts:
                toks.append(P.dma("gpsimd", dst, ya[b][0:nrows, :], s_o[b], waits=[t_y], store=True))
            if hook is not None:
                toks.append(hook(P, hs, t, ya[b], t_y))
            y_free[b] = toks
        P.flush()


def v1_phase(nc, name, jobs, flags_in):
    with Phase(nc, name) as P:
        fl = P.sb("fl", [128, 16], F32)
        xa = [P.sb(f"xa{i}", [128, D], F32) for i in range(2)]
        va = [P.sb(f"va{i}", [128, NH * VS], BF16) for i in range(2)]
        s_c = P.dsem("c")
        s_x = [P.dsem(f"x{i}") for i in range(2)]
        s_o = [P.dsem(f"o{i}") for i in range(2)]
        t_c = P.dma("sync", fl[:], flags_in[:, :], s_c)
        x_free = [None, None]
        v_free = [None, None]
        for t, (src, dst, fcol) in enumerate(jobs):
            b = t % 2
            t_x = P.dma("sync", xa[b][:], src, s_x[b], waits=[x_free[b]])
            v3 = va[b][:].rearrange("p (h c) -> p h c", c=VS)
            t_0 = P.op("gpsimd", lambda e, v3=v3: e.memset(v3[:, :, 65:VS], 0.0), waits=[v_free[b]], sig=True)
            t_1 = P.op("vector", lambda e, b=b, v3=v3: e.tensor_copy(out=v3[:, :, 0:64],
                                                                    in_=xa[b][:].rearrange("p (h c) -> p h c", c=64)),
                       waits=[t_x, v_free[b]], sig=True)
            if fcol is None:
                t_2 = P.op("gpsimd", lambda e, v3=v3: e.memset(v3[:, :, 64:65], 1.0), waits=[v_free[b], t_0], sig=True)
            else:
                t_2 = P.op("gpsimd", lambda e, v3=v3, fcol=fcol: e.tensor_copy(
                    out=v3[:, :, 64:65], in_=fl[:, fcol:fcol + 1].unsqueeze(1).to_broadcast([128, NH, 1])),
                    waits=[v_free[b], t_c, t_0], sig=True)
            x_free[b] = t_1
            v_free[b] = P.dma("gpsimd", dst, va[b][:], s_o[b], waits=[t_1, t_2], store=True)
        P.flush()


def attn_phase(nc, name, units, ident_in, bt_in, QT, KT, V1, OA):
    with Phase(nc, name) as P:
        identf = P.sb("identf", [128, 128], F32)
        identb = P.sb("identb", [128, 128], BF16)
        btf = P.sb("btf", [128, 1024], F32)
        btb = P.sb("btb", [128, 5, NH * 128], BF16)
        qe = [P.sb(f"qe{i}", [128, KC * 128], BF16) for i in range(2)]
        qo = [P.sb(f"qo{i}", [128, KC * 128], BF16) for i in range(2)]
        kt = [P.sb(f"kt{i}", [128, 5, KC * 128], BF16) for i in range(2)]
        v1 = [P.sb(f"v1{i}", [128, 5, NH * VS], BF16) for i in range(2)]
        et = [P.sb(f"et{i}", [128, 5, 512], BF16) for i in range(2)]
        osb = [P.sb(f"osb{i}", [128, NH * 65], F32) for i in range(2)]
        rc = [P.sb(f"rc{i}", [128, NH], F32) for i in range(2)]
        oa = [P.sb(f"oa{i}", [128, D], F32) for i in range(2)]
        pS = [P.ps(f"pS{i}", [128, 512], F32) for i in range(3)]
        pO = [P.ps(f"pO{i}", [128, 512], F32) for i in range(2)]
        s_c = P.dsem("c")
        s_bt = P.dsem("bt")
        s_q = [P.dsem(f"q{i}") for i in range(2)]
        s_k = [P.dsem(f"k{i}") for i in range(2)]
        s_v = [P.dsem(f"v{i}") for i in range(2)]
        s_o = [P.dsem(f"o{i}") for i in range(2)]
        t_id = P.dma("sync", identf[:], ident_in[:, :], s_c)
        t_idb = P.op("vector", lambda e: e.tensor_copy(out=identb[:], in_=identf[:]), waits=[t_id], sig=True)
        t_z = None
        for i in range(2):
            P.op("gpsimd", lambda e, i=i: e.memset(qe[i][64:128, :], 0.0))
            t_z = P.op("gpsimd", lambda e, i=i: e.memset(qo[i][0:64, :], 0.0), sig=True)
        t_bt = None
        for kb in range(5):
            for c in range(4):
                t_l = P.dma("sync", btf[:], bt_in[kb, :, c * 1024:(c + 1) * 1024], s_bt, waits=[t_bt])
                t_bt = P.op("vector", lambda e, kb=kb, c=c: e.tensor_copy(out=btb[:, kb, c * 1024:(c + 1) * 1024], in_=btf[:]),
                            waits=[t_l], sig=True)
        q_free = [None, None]
        k_free = [None, None]
        v_free = [None, None]
        et_free = [None, None]
        osb_free = [None, None]
        oa_free = [None, None]
        pS_free = [None] * 3
        pO_free = [None] * 2
        si = 0
        gi = 0
        for u, (tq, ktiles) in enumerate(units):
            b = u % 2
            P.dma("sync", qe[b][0:64, :], QT[tq][0:64, :], s_q[b], waits=[q_free[b], t_z])
            t_q = P.dma("sync", qo[b][64:128, :], QT[tq][64:128, :], s_q[b], waits=[q_free[b], t_z])
            t_k = None
            t_v = None
            for j, tk_ in enumerate(ktiles):
                t_k = P.dma("sync", kt[b][:, j, :], KT[tk_], s_k[b], waits=[k_free[b]] if j == 0 else [])
                t_v = P.dma("sync", v1[b][:, j, :], V1[tk_], s_v[b], waits=[v_free[b]] if j == 0 else [])
            last_pv = None
            ev_toks = []
            for hg in range(8):
                eb = gi % 2
                pob = gi % 2
                gi += 1
                exp_toks = []
                for kb in range(5):
                    sb_ = si % 3
                    si += 1
                    P.op("tensor", lambda e, sb_=sb_, kb=kb, hg=hg: e.matmul(
                        pS[sb_][:], identb[:], btb[:, kb, hg * 512:(hg + 1) * 512], start=True, stop=False),
                        waits=[pS_free[sb_], t_bt, t_idb, t_q, t_k])
                    for hh in range(4):
                        h = hg * 4 + hh
                        kc, p0 = h // 2, (h % 2) * 64
                        qsel = qe if p0 == 0 else qo
                        t_s = P.op("tensor", lambda e, sb_=sb_, kb=kb, hh=hh, kc=kc, qsel=qsel, b=b: e.matmul(
                            pS[sb_][:, hh * 128:(hh + 1) * 128],
                            kt[b][:, kb, kc * 128:(kc + 1) * 128],
                            qsel[b][:, kc * 128:(kc + 1) * 128], start=False, stop=(hh == 3)),
                            sig=(hh == 3))
                    t_e = P.op("scalar", lambda e, sb_=sb_, kb=kb, eb=eb: e.activation(
                        out=et[eb][:, kb, :], in_=pS[sb_][:], func=AF.Exp),
                        waits=[t_s, et_free[eb]] if kb == 0 else [t_s], sig=True)
                    pS_free[sb_] = t_e
                    exp_toks.append(t_e)
                for hh in range(4):
                    h = hg * 4 + hh
                    for kb in range(5):
                        if ATT_STAGE < 2 and not (hh == 3 and kb == 4):
                            continue
                        last_pv = P.op("tensor", lambda e, pob=pob, hh=hh, kb=kb, h=h, eb=eb, b=b: e.matmul(
                            pO[pob][:, hh * 65:(hh + 1) * 65], et[eb][:, kb, hh * 128:(hh + 1) * 128],
                            v1[b][:, kb, h * VS:h * VS + 65], start=(kb == 0), stop=(kb == 4)),
                            waits=(exp_toks + [pO_free[pob], t_v]) if (hh == 0 and kb == 0) else [],
                            sig=(hh == 3 and kb == 4))
                et_free[eb] = last_pv
                t_ev = P.op("vector", lambda e, pob=pob, hg=hg, b=b: e.tensor_copy(
                    out=osb[b][:, hg * 260:(hg + 1) * 260], in_=pO[pob][:, 0:260]),
                    waits=[last_pv, osb_free[b]] if hg == 0 else [last_pv], sig=True)
                pO_free[pob] = t_ev
                ev_toks.append(t_ev)
            q_free[b] = last_pv
            k_free[b] = last_pv
            v_free[b] = last_pv
            o3 = osb[b][:].rearrange("p (h c) -> p h c", c=65)
            if ATT_STAGE < 3:
                t_n = P.op("vector", lambda e, b=b: e.tensor_copy(out=oa[b][:], in_=osb[b][:, 0:D]),
                           waits=ev_toks + [oa_free[b]], sig=True)
                osb_free[b] = t_n
                oa_free[b] = P.dma("gpsimd", OA[tq * 128:(tq + 1) * 128, :], oa[b][:], s_o[b], waits=[t_n], store=True)
                continue
            t_r0 = P.op("vector", lambda e, b=b, o3=o3: e.tensor_scalar(out=rc[b][:].unsqueeze(2), in0=o3[:, :, 64:65], scalar1=1e-30,
                                                                        scalar2=None, op0=ALU.max), waits=ev_toks, sig=True)
            t_r = P.op("vector", lambda e, b=b: e.reciprocal(out=rc[b][:], in_=rc[b][:]), waits=[t_r0], sig=True)
            t_n = P.op("vector", lambda e, b=b, o3=o3: e.tensor_tensor(
                out=oa[b][:].rearrange("p (h c) -> p h c", c=64), in0=o3[:, :, 0:64],
                in1=rc[b][:].unsqueeze(2).to_broadcast([128, NH, 64]), op=ALU.mult),
                waits=[t_r, oa_free[b]], sig=True)
            osb_free[b] = t_n
            oa_free[b] = P.dma("gpsimd", OA[tq * 128:(tq + 1) * 128, :], oa[b][:], s_o[b], waits=[t_n], store=True)
        P.flush()


class SwiGLUEpi:
    def __init__(self, H):
        self.H = H

    def setup(self, P):
        self.sg = [P.sb(f"sg{i}", [128, 512], F32) for i in range(3)]
        self.ob = [P.sb(f"ob{i}", [128, 512], F32) for i in range(3)]
        self.so = [P.dsem(f"so{i}") for i in range(3)]
        self.free = [None] * 3
        self.sgfree = [None] * 3
        self.i = 0

    def __call__(self, P, tile, blk, pss, ready):
        k = self.i % 3
        self.i += 1
        t_s = P.op("scalar", lambda e: e.activation(out=self.sg[k][:], in_=pss[0][:], func=AF.Silu),
                   waits=[ready, self.sgfree[k]], sig=True)
        t_h = P.op("vector", lambda e: e.tensor_tensor(out=self.ob[k][:], in0=self.sg[k][:], in1=pss[1][:], op=ALU.mult),
                   waits=[t_s, self.free[k]], sig=True)
        self.sgfree[k] = t_h
        self.free[k] = P.dma("gpsimd", self.H[tile * 128:(tile + 1) * 128, blk * 512:(blk + 1) * 512], self.ob[k][:],
                             self.so[k], waits=[t_h], store=True)
        return [t_h]


def copy_phase(nc, name, pairs):
    with Phase(nc, name) as P:
        s = P.dsem("c")
        for (dst, src) in pairs:
            P.dma("sync", dst, src, s, store=True)
        P.flush()


def scan_phase(nc, name, seqs, UT, GGT, XTout, STd, pf_in, w_rg_a, w_rg_x, emit):
    SEG = 512
    with Phase(nc, name) as P:
        pf = P.sb("pf", [128, 8, KC], F32)
        cl = P.sb("cl", [128, KC], F32)
        wst = P.sb("wst", [128, 2, 256], F32)
        wa = [P.sb(f"wa{i}", [128, 2, 256], BF16) for i in range(2)]
        wx = [P.sb(f"wx{i}", [128, 2, 256], BF16) for i in range(2)]
        ub = [P.sb(f"ub{i}", [128, 2, 3 + SEG], F32) for i in range(2)]
        gb = [P.sb(f"gb{i}", [128, 2, SEG], F32) for i in range(2)]
        uc = [P.sb(f"uc{i}", [128, 2, SEG], F32) for i in range(2)]
        ucb = [P.sb(f"ucb{i}", [128, 2, SEG], BF16) for i in range(2)]
        rr = P.sb("rr", [128, 2, SEG], F32)
        ii = P.sb("ii", [128, 2, SEG], F32)
        aa = P.sb("aa", [128, 2, SEG], F32)
        ss = P.sb("ss", [128, 2, SEG], F32)
        hh = P.sb("hh", [128, 2, SEG], F32)
        yb = [P.sb(f"yb{i}", [128, 2, SEG], BF16) for i in range(2)]
        carry = P.sb("carry", [128, 2], F32)
        stt = [P.sb(f"stt{i}", [128, 2, 4], F32) for i in range(2)]
        pr = [P.ps(f"pr{i}", [128, SEG], F32) for i in range(4)]
        s_c = P.dsem("c")
        s_w = P.dsem("w")
        s_u = [P.dsem(f"u{i}") for i in range(2)]
        s_g = [P.dsem(f"g{i}") for i in range(2)]
        s_h = P.dsem("h")
        s_y = [[P.dsem(), P.dsem()] for i in range(2)]
        s_s = [P.dsem(f"s{i}") for i in range(2)]
        t_pf = P.dma("sync", pf[:], pf_in[:, :, :], s_c)
        t0 = P.op("scalar", lambda e: e.activation(out=cl[:], in_=pf[:, 7, :], func=AF.Exp, scale=-1.0), waits=[t_pf], sig=True)
        t1 = P.op("scalar", lambda e: e.activation(out=cl[:], in_=cl[:], func=AF.Ln, bias=1.0), waits=[t0], sig=True)
        t_cl = P.op("vector", lambda e: e.tensor_scalar(out=cl[:], in0=cl[:], scalar1=-8.0, scalar2=None, op0=ALU.mult),
                    waits=[t1], sig=True)
        wst_free = None
        w_free = [None, None]
        u_free = [None, None]
        g_free = [None, None]
        y_free = [None, None]
        st_free = [None, None]
        pr_free = [None] * 4
        tail = None
        it = 0
        sti = 0
        for n in range(8):
            wbuf = n % 2
            t_l = P.dma("sync", wst[:], w_rg_a[n].rearrange("(k p) c -> p k c", p=128), s_w, waits=[wst_free])
            t_wa = P.op("vector", lambda e, wbuf=wbuf: e.tensor_copy(out=wa[wbuf][:], in_=wst[:]), waits=[t_l, w_free[wbuf]], sig=True)
            t_l = P.dma("sync", wst[:], w_rg_x[n].rearrange("(k p) c -> p k c", p=128), s_w, waits=[t_wa])
            t_wx = P.op("vector", lambda e, wbuf=wbuf: e.tensor_copy(out=wx[wbuf][:], in_=wst[:]), waits=[t_l, w_free[wbuf]], sig=True)
            wst_free = t_wx
            last_mm = None
            for sq in seqs:
                L_tot = sq["L"]
                t_h0 = P.dma("sync", carry[:].unsqueeze(2), sq["h0"][:, 2 * n:2 * n + 2, :], s_h, waits=[tail])
                nseg = (L_tot + SEG - 1) // SEG
                for sg in range(nseg):
                    b = it % 2
                    it += 1
                    c0 = sq["col0"] + sg * SEG
                    L = min(SEG, L_tot - sg * SEG)
                    halo = sq["halo"][:, 2 * n:2 * n + 2, :] if sg == 0 else UT[:, 2 * n:2 * n + 2, c0 - 3:c0]
                    t_u1 = P.dma("sync", ub[b][:, :, 0:3], halo, s_u[b], waits=[u_free[b]])
                    t_u = P.dma("sync", ub[b][:, :, 3:3 + L], UT[:, 2 * n:2 * n + 2, c0:c0 + L], s_u[b], waits=[u_free[b]])
                    if emit:
                        t_g = P.dma("sync", gb[b][:, :, 0:L], GGT[:, 2 * n:2 * n + 2, c0:c0 + L], s_g[b], waits=[g_free[b]])
                    tc = None
                    for j in range(2):
                        kc = 2 * n + j
                        tc = P.op("vector", lambda e, b=b, j=j, kc=kc, L=L: e.tensor_scalar(
                            out=uc[b][:, j, 0:L], in0=ub[b][:, j, 0:L], scalar1=pf[:, 0, kc:kc + 1], scalar2=pf[:, 4, kc:kc + 1],
                            op0=ALU.mult, op1=ALU.add), waits=[t_u, t_pf, tc, tail] if j == 0 else [tc], sig=True)
                        for i in range(1, 4):
                            tc = P.op("vector", lambda e, b=b, j=j, kc=kc, L=L, i=i: e.scalar_tensor_tensor(
                                out=uc[b][:, j, 0:L], in0=ub[b][:, j, i:i + L], scalar=pf[:, i, kc:kc + 1], in1=uc[b][:, j, 0:L],
                                op0=ALU.mult, op1=ALU.add), waits=[tc], sig=True)
                    t_cb = P.op("vector", lambda e, b=b, L=L: e.tensor_copy(out=ucb[b][:, :, 0:L], in_=uc[b][:, :, 0:L]),
                                waits=[tc, last_mm], sig=True)
                    gate_toks = []
                    for gsel, (wt, dstt, brow) in enumerate(((wa, rr, 5), (wx, ii, 6))):
                        for j in range(2):
                            pk = gsel * 2 + j
                            for ci in range(2):
                                last_mm = P.op("tensor", lambda e, pk=pk, wt=wt, wbuf=wbuf, ci=ci, j=j, b=b, L=L: e.matmul(
                                    pr[pk][:, 0:L], wt[wbuf][:, ci, j * 128:(j + 1) * 128], ucb[b][:, ci, 0:L],
                                    start=(ci == 0), stop=(ci == 1)),
                                    waits=[t_cb, t_wa, t_wx, pr_free[pk]] if ci == 0 else [], sig=(ci == 1))
                            kc = 2 * n + j
                            tg_ = P.op("scalar", lambda e, pk=pk, dstt=dstt, j=j, kc=kc, brow=brow, L=L: e.activation(
                                out=dstt[:, j, 0:L], in_=pr[pk][:, 0:L], func=AF.Sigmoid, bias=pf[:, brow, kc:kc + 1]),
                                waits=[last_mm, tail], sig=True)
                            pr_free[pk] = tg_
                            gate_toks.append(tg_)
                    ta = None
                    for j in range(2):
                        kc = 2 * n + j
                        ta = P.op("scalar", lambda e, j=j, kc=kc, L=L: e.activation(
                            out=aa[:, j, 0:L], in_=rr[:, j, 0:L], func=AF.Exp, scale=cl[:, kc:kc + 1]),
                            waits=gate_toks + [t_cl, tail], sig=True)
                    t_a2 = P.op("vector", lambda e, L=L: e.tensor_tensor(out=ss[:, :, 0:L], in0=aa[:, :, 0:L], in1=aa[:, :, 0:L],
                                                                         op=ALU.mult), waits=[ta, tail], sig=True)
                    t_sq = P.op("scalar", lambda e, L=L: e.activation(out=ss[:, :, 0:L], in_=ss[:, :, 0:L], func=AF.Sqrt,
                                                                      scale=-1.0, bias=1.0), waits=[t_a2], sig=True)
                    t_b1 = P.op("vector", lambda e, b=b, L=L: e.tensor_tensor(out=ii[:, :, 0:L], in0=ii[:, :, 0:L], in1=uc[b][:, :, 0:L],
                                                                             op=ALU.mult), waits=gate_toks + [t_a2], sig=True)
                    t_b2 = P.op("vector", lambda e, L=L: e.tensor_tensor(out=ii[:, :, 0:L], in0=ii[:, :, 0:L], in1=ss[:, :, 0:L],
                                                                        op=ALU.mult), waits=[t_b1, t_sq], sig=True)
                    th = t_b2
                    for j in range(2):
                        th = P.op("vector", lambda e, j=j, L=L: e.tensor_tensor_scan(
                            out=hh[:, j, 0:L], data0=aa[:, j, 0:L], data1=ii[:, j, 0:L], initial=carry[:, j:j + 1],
                            op0=ALU.mult, op1=ALU.add), waits=[th, t_h0], sig=True)
                    t_cy = P.op("vector", lambda e, L=L: e.tensor_copy(out=carry[:].unsqueeze(2), in_=hh[:, :, L - 1:L]),
                                waits=[th], sig=True)
                    tail = t_cy
                    if emit:
                        t_y = P.op("vector", lambda e, b=b, L=L: e.tensor_tensor(out=yb[b][:, :, 0:L], in0=hh[:, :, 0:L],
                                                                                in1=gb[b][:, :, 0:L], op=ALU.mult),
                                   waits=[t_cy, t_g, y_free[b]], sig=True)
                        tail = t_y
                        g_free[b] = t_y
                        t0_ = c0 // 128
                        nt = (L + 127) // 128
                        toks = []
                        for j in range(2):
                            kc = 2 * n + j
                            if L % 128 == 0:
                                dstv = XTout[t0_:t0_ + nt, :, kc * 128:(kc + 1) * 128].rearrange("t p c -> p t c")
                                srcv = yb[b][:, j, 0:L].rearrange("p (t c) -> p t c", c=128)
                            else:
                                dstv = XTout[t0_][:, kc * 128:kc * 128 + L]
                                srcv = yb[b][:, j, 0:L]
                            toks.append(P.dma("gpsimd", dstv, srcv, s_y[b][j], waits=[t_y], store=True))
                        y_free[b] = toks
                    u_free[b] = tail
                    if sg == nseg - 1:
                        k = sti % 2
                        sti += 1
                        t_s1 = P.op("gpsimd", lambda e, k=k, b=b, L=L: e.tensor_copy(out=stt[k][:, :, 0:3], in_=ub[b][:, :, L:L + 3]),
                                    waits=[t_u, t_u1, st_free[k]], sig=True)
                        t_s2 = P.op("gpsimd", lambda e, k=k, L=L: e.tensor_copy(out=stt[k][:, :, 3:4], in_=hh[:, :, L - 1:L]),
                                    waits=[th, t_s1], sig=True)
                        tail = [tail, t_s2]
                        u_free[b] = tail
                        st_free[k] = P.dma("gpsimd", STd[sq["st"]][:, 2 * n:2 * n + 2, 0:4], stt[k][:], s_s[k], waits=[t_s2], store=True)
            w_free[wbuf] = last_mm
        P.flush()


def carry_phase(nc, name, STd, flags_in, HALOd, H0d):
    with Phase(nc, name) as P:
        st = P.sb("st", [128, KC, 4], F32)
        fl = P.sb("fl", [128, 16], F32)
        s1, s2, s3, s4 = P.dsem(), P.dsem(), P.dsem(), P.dsem()
        t_f = P.dma("sync", fl[:], flags_in[:, :], s1)
        t_a = P.dma("sync", st[:], STd[0][:, :, 0:4], s2)
        stf = st[:].rearrange("p k c -> p (k c)")
        t_x = P.op("vector", lambda e: e.tensor_scalar(out=stf, in0=stf, scalar1=fl[:, 0:1], scalar2=None, op0=ALU.mult),
                   waits=[t_f, t_a], sig=True)
        P.dma("gpsimd", HALOd[:, :, :], st[:, :, 0:3], s3, waits=[t_x], store=True)
        P.dma("gpsimd", H0d[:, :, :], st[:, :, 3:4], s4, waits=[t_x], store=True)
        P.flush()


def exchange_phase(nc, name, STd, CCin, CCout, flags_in, UT, H0d):
    with Phase(nc, name) as P:
        st = P.sb("st", [128, KC, 4], F32)
        g8 = P.sb("g8", [128, 2, KC * 4], F32)
        fl = P.sb("fl", [128, 16], F32)
        acc = P.sb("acc", [128, KC, 4], F32)
        s1, s2, s4, s5 = P.dsem(), P.dsem(), P.dsem(), P.dsem()
        s3 = P.G.cc
        s6, s7 = P.dsem(), P.dsem()
        t_f = P.dma("sync", fl[:], flags_in[:, :], s5)
        t_a = P.dma("sync", st[:], STd[0][:, :, 0:4], s1)
        t_b = P.dma("sync", CCin[:, :], st[:].rearrange("p k c -> p (k c)"), s2, waits=[t_a])
        tok = s3.next()
        P.q["gpsimd"].append(Op(_flat([t_b]), lambda e: e.collective_compute(
            "AllGather", ALU.bypass, replica_groups=[[0, 1], [2, 3], [4, 5], [6, 7]], ins=[CCin[:, :]], outs=[CCout[:, :]]), s3))
        t_g = P.dma("sync", g8[:], CCout.rearrange("(r p) c -> p r c", p=128), s4, waits=[tok])
        accf = acc[:].rearrange("p k c -> p (k c)")
        t_x = P.op("vector", lambda e: e.tensor_scalar(out=accf, in0=g8[:, 0, :], scalar1=fl[:, 2:3], scalar2=None, op0=ALU.mult),
                   waits=[t_g, t_f], sig=True)
        for r in range(1, 2):
            t_x = P.op("vector", lambda e, r=r: e.scalar_tensor_tensor(out=accf, in0=g8[:, r, :], scalar=fl[:, 2 + r:3 + r], in1=accf,
                                                                       op0=ALU.mult, op1=ALU.add), waits=[t_x], sig=True)
        P.dma("gpsimd", UT[:, :, 509:512], acc[:, :, 0:3], s6, waits=[t_x], store=True)
        P.dma("gpsimd", H0d[:, :, :], acc[:, :, 3:4], s7, waits=[t_x], store=True)
        P.flush()


def router_setup_factory(wrT_in):
    def setup(P):
        st = dict(wr=P.sb("wr", [128, NEXP, D], F32), junk=P.sb("junk", [128, D], F32),
                  lg=[P.sb(f"lg{i}", [128, 8], F32) for i in range(2)], m8=[P.sb(f"m8{i}", [128, 8], F32) for i in range(2)],
                  m1=[P.sb(f"m1{i}", [128, 8], F32) for i in range(2)], m2=[P.sb(f"m2{i}", [128, 8], F32) for i in range(2)],
                  dl=[P.sb(f"dl{i}", [128, 4], F32) for i in range(2)], gs=[P.sb(f"gs{i}", [128, 8], F32) for i in range(2)],
                  sw=P.dsem("wr"), so=[P.dsem(f"rg{i}") for i in range(2)], free=[None, None], tw=None, last=None)
        for e in range(NEXP):
            st["tw"] = P.dma("sync", st["wr"][:, e, :], wrT_in[e:e + 1, :].to_broadcast([128, D]), st["sw"])
        return st
    return setup


def make_router_hook(GATES, tiles):
    def hook(P, st, t, y, t_y):
        b = t % 2
        lg, m8, m1, m2, dl, gs = st["lg"][b], st["m8"][b], st["m1"][b], st["m2"][b], st["dl"][b], st["gs"][b]
        tk = st["last"]
        for e in range(NEXP):
            tk = P.op("vector", lambda en, e=e: en.scalar_tensor_tensor(
                out=st["junk"][:], in0=y[:], scalar=1.0, in1=st["wr"][:, e, :], op0=ALU.mult, op1=ALU.mult,
                accum_out=lg[:, e:e + 1]), waits=[t_y, st["tw"], tk, st["free"][b]], sig=True)
        t8 = P.op("vector", lambda en: en.max(out=m8[:], in_=lg[:]), waits=[tk], sig=True)
        t1 = P.op("vector", lambda en: en.tensor_scalar(out=m1[:], in0=lg[:], scalar1=m8[:, 0:1], scalar2=None, op0=ALU.is_equal),
                  waits=[t8], sig=True)
        t2 = P.op("vector", lambda en: en.tensor_scalar(out=m2[:], in0=lg[:], scalar1=m8[:, 1:2], scalar2=None, op0=ALU.is_equal),
                  waits=[t1], sig=True)
        t3 = P.op("vector", lambda en: en.tensor_tensor(out=dl[:, 0:1], in0=m8[:, 0:1], in1=m8[:, 1:2], op=ALU.subtract),
                  waits=[t2], sig=True)
        t4 = P.op("scalar", lambda en: en.activation(out=dl[:, 1:2], in_=dl[:, 0:1], func=AF.Sigmoid), waits=[t3], sig=True)
        t5 = P.op("scalar", lambda en: en.activation(out=dl[:, 2:3], in_=dl[:, 0:1], func=AF.Sigmoid, scale=-1.0), waits=[t4], sig=True)
        t6 = P.op("vector", lambda en: en.tensor_scalar(out=gs[:], in0=m1[:], scalar1=dl[:, 1:2], scalar2=None, op0=ALU.mult),
                  waits=[t5], sig=True)
        t7 = P.op("vector", lambda en: en.scalar_tensor_tensor(out=gs[:], in0=m2[:], scalar=dl[:, 2:3], in1=gs[:], op0=ALU.mult,
                                                              op1=ALU.add), waits=[t6], sig=True)
        st["last"] = t7
        tile = tiles[t]
        st["free"][b] = P.dma("gpsimd", GATES[tile * 128:(tile + 1) * 128, :], gs[:], st["so"][b], waits=[t7], store=True)
        return [t7, st["free"][b]]
    return hook


def build_program(S_OWN, upto=99):
    assert S_OWN % 512 == 0
    NX = 512 + 2 * S_OWN + 256
    NXT = NX // 128
    NPAIR = S_OWN // 128
    RT = list(range(4, NXT))
    RT2 = list(range(4 + NPAIR, NXT))
    TS = [4 + 2 * NPAIR, 4 + 2 * NPAIR + 1]
    OWN0 = 512 + S_OWN

    nc = bass.Bass("TRN2", target_bir_lowering=False)

    def din(name, shape, dt=F32):
        return nc.dram_tensor(name, list(shape), dt, kind="ExternalInput").ap()

    def dout(name, shape, dt=F32):
        return nc.dram_tensor(name, list(shape), dt, kind="ExternalOutput").ap()

    def dscr(name, shape, dt):
        return nc.dram_tensor(name, list(shape), dt, kind="ExternalOutput" if DEBUG else "Internal").ap()

    xin = din("xin", [NX, D])
    cache_k = din("cache_k", [2, 512, D])
    cache_v = din("cache_v", [2, 512, D])
    ident_in = din("ident", [128, 128])
    bt_in = din("bt", [5, 128, NH * 128])
    flags = din("flags", [128, 16])
    ln_g = din("ln_g", [4, D])
    ln_b = din("ln_b", [4, D])
    w_attn_in = din("w_attn_in", [D, 3 * D])
    w_attn_out = din("w_attn_out", [D, D])
    w_ffn_in = din("w_ffn_in", [D, 2 * DFF])
    w_ffn_out = din("w_ffn_out", [DFF, D])
    w_rnn_in = din("w_rnn_in", [D, 2 * D])
    w_rnn_out = din("w_rnn_out", [D, D])
    w_rg_a = din("w_rg_a", [8, 256, 256])
    w_rg_x = din("w_rg_x", [8, 256, 256])
    pf_in = din("pf", [128, 8, KC])
    sconv_in = din("sconv", [256, D])
    zeros_in = din("zeros", [128, D])
    wrT_in = din("wrT", [NEXP, D])
    if upto >= 8:
        w_moe_in = din("w_moe_in", [NEXP, D, 2 * DFF])
        w_moe_out = din("w_moe_out", [NEXP, DFF, D])

    o_newk = dout("o_newk", [512 + 32, D])
    o_newv = dout("o_newv", [512 + 32, D])
    o_y = dout("o_y", [S_OWN + 256, D])
    o_state = dout("o_state", [3, 4, D])

    XT = dscr("XT", [NXT, 128, KC * 128], BF16)
    QKV = dscr("QKV", [NX, 3 * D], F32)
    QT = dscr("QT", [NXT, 128, KC * 128], BF16)
    KT = dscr("KT", [NXT + 8, 128, KC * 128], BF16)
    V1 = dscr("V1", [NXT + 8, 128, NH * VS], BF16)
    OA = dscr("OA", [NX, D], F32)
    M = dscr("M", [NX, D], F32)
    X1 = dscr("X1", [NX, D], F32)
    X2 = dscr("X2", [NX, D], F32)
    H = dscr("H", [NX, DFF], F32)
    HT = dscr("HT", [NXT, 128, 44 * 128], BF16)
    X3 = dscr("X3", [NX, D], F32)
    UG = dscr("UG", [NX, 2 * D], F32)
    UT = dscr("UT", [128, KC, NX], F32)
    GGT = dscr("GGT", [128, KC, NX], F32)
    SCT = dscr("SCT", [128, KC, 256], F32)
    STd = dscr("STd", [3, 128, KC, 128], F32)
    STo = dscr("STo", [3, 128, D], F32)
    H0d = dscr("H0d", [128, KC, 1], F32)
    HALOd = dscr("HALOd", [128, KC, 3], F32)
    CCin = nc.dram_tensor("CCin", [128, KC * 4], F32, kind="Internal").ap()
    CCout = nc.dram_tensor("CCout", [2 * 128, KC * 4], F32, kind="Internal").ap()
    GATES = dscr("GATES", [NX, 8], F32)
    FA = dscr("FA", [NX, D], F32)

    def rows(ap, t, c0=0, c1=None):
        return ap[t * 128:(t + 1) * 128, c0:(c1 if c1 is not None else ap.shape[1])]

    transpose_phase(nc, "t0", ident_in, [(rows(xin, t), XT[t]) for t in range(NXT)], KC)
    if upto >= 2:
        gemm_phase(nc, "g0", XT, list(range(NXT)), KC, w_attn_in, [[c] for c in range(0, 3 * D, 512)],
                   StoreEpi(QKV, lambda blk, s: blk * 512))
    if upto >= 3:
        transpose_phase(nc, "tq", ident_in, [(rows(QKV, t, 0, D), QT[t]) for t in RT], KC, scale=0.125)
        jobs = [(rows(QKV, t, D, 2 * D), KT[t]) for t in range(NXT)]
        jobs += [(cache_k[s, j * 128:(j + 1) * 128, :], KT[NXT + 4 * s + j]) for s in range(2) for j in range(4)]
        transpose_phase(nc, "tk", ident_in, jobs, KC)
        jobs = [(rows(QKV, t, 2 * D, 3 * D), V1[t], (3 if t < 4 else (0 if t < 4 + NPAIR else (1 if t in TS else None))))
                for t in range(NXT)]
        jobs += [(cache_v[s, j * 128:(j + 1) * 128, :], V1[NXT + 4 * s + j], None) for s in range(2) for j in range(4)]
        v1_phase(nc, "v1", jobs, flags)
        r0 = OWN0 + S_OWN - 512
        cp = [(o_newk[0:512, :], QKV[r0:r0 + 512, D:2 * D]), (o_newv[0:512, :], QKV[r0:r0 + 512, 2 * D:3 * D])]
        for s in range(2):
            rs = TS[s] * 128
            cp.append((o_newk[512 + 16 * s:528 + 16 * s, :], QKV[rs:rs + 16, D:2 * D]))
            cp.append((o_newv[512 + 16 * s:528 + 16 * s, :], QKV[rs:rs + 16, 2 * D:3 * D]))
        copy_phase(nc, "cpkv", cp)
    if upto >= 4:
        units = [(4 + u, [u + j for j in range(5)]) for u in range(2 * NPAIR)]
        units += [(TS[s], [NXT + 4 * s + j for j in range(4)] + [TS[s]]) for s in range(2)]
        attn_phase(nc, "att", units, ident_in, bt_in, QT, KT, V1, OA)
    if upto >= 5:
        transpose_phase(nc, "toa", ident_in, [(rows(OA, t), XT[t]) for t in RT], KC)
        gemm_phase(nc, "go", XT, RT, KC, w_attn_out, [[c] for c in range(0, D, 512)], StoreEpi(M, lambda blk, s: blk * 512))
        ln_phase(nc, "ln0", [(rows(xin, t), rows(M, t), [(rows(X1, t), 128)]) for t in RT], ln_g[0:1, :], ln_b[0:1, :])
    if upto >= 6:
        transpose_phase(nc, "tx1", ident_in, [(rows(X1, t), XT[t]) for t in RT], KC)
        gemm_phase(nc, "gf1", XT, RT, KC, w_ffn_in, [[c, DFF + c] for c in range(0, DFF, 512)], SwiGLUEpi(H))
        transpose_phase(nc, "th", ident_in, [(rows(H, t), HT[t]) for t in RT], 44)
        gemm_phase(nc, "gf2", HT, RT, 44, w_ffn_out, [[c] for c in range(0, D, 512)], StoreEpi(M, lambda blk, s: blk * 512))
        ln_phase(nc, "ln1", [(rows(X1, t), rows(M, t), [(rows(X2, t), 128)]) for t in RT], ln_g[1:2, :], ln_b[1:2, :])
    if upto >= 7:
        transpose_phase(nc, "tx2", ident_in, [(rows(X2, t), XT[t]) for t in RT], KC)
        gemm_phase(nc, "gr1", XT, RT, KC, w_rnn_in, [[c] for c in range(0, 2 * D, 512)], StoreEpi(UG, lambda blk, s: blk * 512))
        jobs = [(rows(UG, t, 0, D), UT[:, :, t * 128:(t + 1) * 128]) for t in RT]
        transpose_phase(nc, "tu", ident_in, jobs, KC, out_dt=F32, dst3=True)
        jobs = [(None, GGT[:, :, t * 128:(t + 1) * 128]) for t in RT2]
        transpose_phase(nc, "tg", ident_in, jobs, KC, out_dt=F32, dst3=True,
                        prep=make_gelu_prep([rows(UG, t, D, 2 * D) for t in RT2]), prep_setup=gelu_prep_setup)
        jobs = [(sconv_in[s * 128:(s + 1) * 128, :], SCT[:, :, s * 128:(s + 1) * 128]) for s in range(2)]
        transpose_phase(nc, "tsc", ident_in, jobs, KC, out_dt=F32, dst3=True)
        ZER = zeros_in.rearrange("p (k c) -> p k c", c=128)
        copy_phase(nc, "zst", [(STd[i].rearrange("p k c -> p (k c)"), zeros_in[:, :]) for i in range(3)])
        seq_p1 = [dict(col0=512, L=S_OWN, halo=ZER[:, :, 0:3], h0=ZER[:, :, 3:4], st=0)]
        scan_phase(nc, "sc1", seq_p1, UT, GGT, XT, STd, pf_in, w_rg_a, w_rg_x, emit=False)
        carry_phase(nc, "cry", STd, flags, HALOd, H0d)
        seq_p2 = [dict(col0=OWN0, L=S_OWN, halo=HALOd, h0=H0d, st=0)]
        seq_p2 += [dict(col0=TS[s] * 128, L=16, halo=SCT[:, :, s * 128:s * 128 + 3], h0=SCT[:, :, s * 128 + 3:s * 128 + 4], st=1 + s)
                   for s in range(2)]
        scan_phase(nc, "sc2", seq_p2, UT, GGT, XT, STd, pf_in, w_rg_a, w_rg_x, emit=True)
        transpose_phase(nc, "tst", ident_in, [(STd[i].rearrange("p k c -> p (k c)"), STo[i]) for i in range(3)], KC, out_dt=F32)
        copy_phase(nc, "cst", [(o_state[i], STo[i, 0:4, :]) for i in range(3)])
        gemm_phase(nc, "gr2", XT, RT2, KC, w_rnn_out, [[c] for c in range(0, D, 512)], StoreEpi(M, lambda blk, s: blk * 512))
        ln_phase(nc, "ln2", [(rows(X2, t), rows(M, t), [(rows(X3, t), 128)]) for t in RT2], ln_g[2:3, :], ln_b[2:3, :],
                 hook=make_router_hook(GATES, RT2), hook_setup=router_setup_factory(wrT_in))
    if upto >= 8:
        transpose_phase(nc, "tx3", ident_in, [(rows(X3, t), XT[t]) for t in RT2], KC)
        for e in range(NEXP):
            gemm_phase(nc, f"gm1_{e}", XT, RT2, KC, w_moe_in[e], [[c, DFF + c] for c in range(0, DFF, 512)], SwiGLUEpi(H))
            transpose_phase(nc, f"thm{e}", ident_in, [(rows(H, t), HT[t]) for t in RT2], 44)
            gemm_phase(nc, f"gm2_{e}", HT, RT2, 44, w_moe_out[e], [[c] for c in range(0, D, 512)], AccEpi(FA, GATES, e))
        jobs = []
        for t in RT2:
            r0 = (t - 4 - NPAIR) * 128
            jobs.append((rows(X3, t), rows(FA, t), [(o_y[r0:r0 + 128, :], 128)]))
        ln_phase(nc, "ln3", jobs, ln_g[3:4, :], ln_b[3:4, :])
    elif upto >= 7:
        copy_phase(nc, "cpy", [(o_y[:, :], X3[512:NX, :])])
    elif upto >= 6:
        copy_phase(nc, "cpy", [(o_y[:, :], X2[512:NX, :])])
    return nc, dict(NX=NX, NXT=NXT, NPAIR=NPAIR, TS=TS, OWN0=OWN0)


def _bias_table(rel_bias):
    kw = np.arange(640)[:, None]
    qi = np.arange(128)[None, :]
    d = np.clip(qi + 512 - kw, -256, 256) + 256
    tab = rel_bias[:, d]
    tab = np.transpose(tab, (1, 0, 2)).copy()
    dc = (qi // 64 + 8) - kw // 64
    mask = (dc >= 0) & (dc <= 8)
    tab = np.where(mask[:, None, :], tab, np.float32(NEG)).astype(np.float32)
    return np.ascontiguousarray(tab.reshape(5, 128, NH * 128))


def make_in_maps(inp, S_OWN, upto=99):
    xp = inp["x_prompt"]
    xs = inp["x_sample"]
    B = xp.shape[0]
    n_cores = 2 * B
    maps = []
    bt = _bias_table(inp["rel_bias"][0])
    ident = np.eye(128, dtype=np.float32)
    vecs = [inp["conv_w"][0, 0], inp["conv_w"][0, 1], inp["conv_w"][0, 2], inp["conv_w"][0, 3], inp["conv_b"][0],
            inp["b_rg_a"][0].reshape(-1), inp["b_rg_x"][0].reshape(-1), inp["rg_lambda"][0]]
    pf = np.ascontiguousarray(np.stack([v.reshape(KC, 128).T for v in vecs], axis=1)).astype(np.float32)
    zeros = np.zeros((128, D), np.float32)
    wrT = np.ascontiguousarray(inp["w_router"][0].T)
    for c in range(n_cores):
        b, half = c // 2, c % 2
        NX = 512 + 2 * S_OWN + 256
        xin = np.zeros((NX, D), np.float32)
        if half == 1:
            xin[512:512 + S_OWN] = xp[b, 0:S_OWN]
        o0 = 512 + S_OWN
        xin[o0:o0 + S_OWN] = xp[b, half * S_OWN:(half + 1) * S_OWN]
        xin[o0 + S_OWN:o0 + S_OWN + 16] = xs[2 * c]
        xin[o0 + S_OWN + 128:o0 + S_OWN + 144] = xs[2 * c + 1]
        fl = np.zeros((128, 16), np.float32)
        fl[:, 0] = float(half)
        fl[:16, 1] = 1.0
        if half == 1:
            fl[:, 2] = 1.0
        sconv = np.zeros((256, D), np.float32)
        for s_ in range(2):
            sconv[s_ * 128:s_ * 128 + 3] = inp["state_conv"][0, 2 * c + s_]
            sconv[s_ * 128 + 3] = inp["state_h"][0, 2 * c + s_]
        m = {
            "xin": xin,
            "cache_k": np.ascontiguousarray(inp["cache_k"][0, 2 * c:2 * c + 2].reshape(2, 512, D)),
            "cache_v": np.ascontiguousarray(inp["cache_v"][0, 2 * c:2 * c + 2].reshape(2, 512, D)),
            "ident": ident,
            "bt": bt,
            "flags": fl,
            "ln_g": np.ascontiguousarray(inp["ln_g"].reshape(4, D)),
            "ln_b": np.ascontiguousarray(inp["ln_b"].reshape(4, D)),
            "w_attn_in": inp["w_attn_in"][0],
            "w_attn_out": inp["w_attn_out"][0],
            "w_ffn_in": inp["w_ffn_in"][0],
            "w_ffn_out": inp["w_ffn_out"][0],
            "w_rnn_in": inp["w_rnn_in"][0],
            "w_rnn_out": inp["w_rnn_out"][0],
            "w_rg_a": inp["w_rg_a"][0],
            "w_rg_x": inp["w_rg_x"][0],
            "pf": pf,
            "sconv": sconv,
            "zeros": zeros,
            "wrT": wrT,
        }
        if upto >= 8:
            m["w_moe_in"] = inp["w_moe_in"][0]
            m["w_moe_out"] = inp["w_moe_out"][0]
        maps.append(m)
    return maps


def kernel(**inputs):
    inputs = {k: np.asarray(v) for k, v in inputs.items()}
    B, SEQ, _ = inputs["x_prompt"].shape
    S_OWN = SEQ // 2
    upto = int(os.environ.get("MK_UPTO", "8"))
    nc, meta = build_program(S_OWN, upto=upto)
    maps = make_in_maps(inputs, S_OWN, upto)
    res = run_bass_kernel_spmd(nc, maps, core_ids=list(range(8)))
    r = res.results
    NS = inputs["x_sample"].shape[0]
    TSEQ = inputs["x_sample"].shape[1]
    y_prompt = np.zeros((B, SEQ, D), np.float32)
    y_sample = np.zeros((NS, TSEQ, D), np.float32)
    nk_p = np.zeros((1, B, 512, NH, HD), np.float32)
    nv_p = np.zeros((1, B, 512, NH, HD), np.float32)
    nk_s = np.zeros((1, NS, TSEQ, NH, HD), np.float32)
    nv_s = np.zeros((1, NS, TSEQ, NH, HD), np.float32)
    nc_p = np.zeros((1, B, 3, D), np.float32)
    nh_p = np.zeros((1, B, D), np.float32)
    nc_s = np.zeros((1, NS, 3, D), np.float32)
    nh_s = np.zeros((1, NS, D), np.float32)
    for c in range(8):
        b, half = c // 2, c % 2
        y = np.asarray(r[c]["o_y"])
        y_prompt[b, half * S_OWN:(half + 1) * S_OWN] = y[0:S_OWN]
        y_sample[2 * c] = y[S_OWN:S_OWN + 16]
        y_sample[2 * c + 1] = y[S_OWN + 128:S_OWN + 144]
        k = np.asarray(r[c]["o_newk"])
        v = np.asarray(r[c]["o_newv"])
        if half == 1:
            nk_p[0, b] = k[0:512].reshape(512, NH, HD)
            nv_p[0, b] = v[0:512].reshape(512, NH, HD)
        for s in range(2):
            nk_s[0, 2 * c + s] = k[512 + 16 * s:528 + 16 * s].reshape(16, NH, HD)
            nv_s[0, 2 * c + s] = v[512 + 16 * s:528 + 16 * s].reshape(16, NH, HD)
        st = np.asarray(r[c]["o_state"])
        if half == 1:
            nc_p[0, b] = st[0, 0:3]
            nh_p[0, b] = st[0, 3]
        for s in range(2):
            nc_s[0, 2 * c + s] = st[1 + s, 0:3]
            nh_s[0, 2 * c + s] = st[1 + s, 3]
    return (y_prompt, y_sample, nk_p, nv_p, nk_s, nv_s, nc_p, nh_p, nc_s, nh_s)
```
